# Optimizing a Trainium2 kernel written in Bass

```python
import math
import jax, jax.numpy as jnp
from jax import lax
import numpy as np

D_MODEL = 2048
BATCH = 1
SEQ = 8192
DEPTH = 1

POOL_WINDOWS = (2, 4, 8, 16)
N_POOL_GROUPS = len(POOL_WINDOWS)
POOL_WIDTH = D_MODEL // 2
POOL_GROUP_WIDTH = POOL_WIDTH // N_POOL_GROUPS
POOL_OUT_GROUP = D_MODEL // N_POOL_GROUPS
GMLP_HEADS = 8
GMLP_HEAD_DIM = 128
GMLP_WIDTH = GMLP_HEADS * GMLP_HEAD_DIM
CHUNK = 128
IN_WIDTH = POOL_WIDTH + 2 * GMLP_WIDTH + 2 * D_MODEL
PEER_HEADS = 8
N_KEYS = 128
N_EXPERTS = N_KEYS * N_KEYS
QUERY_DIM = 256
HALF_DIM = QUERY_DIM // 2
TOPK = 16
TOKEN_BLOCK = 128
EPS = 1e-6

kernel_name = "hybrid_pool_sgu_peer_block"


def rms_norm(x, g):
    xf = x.astype(jnp.float32)
    y = xf * lax.rsqrt(jnp.mean(xf * xf, axis=-1, keepdims=True) + EPS)
    return (y * g.astype(jnp.float32)).astype(x.dtype)


def layer_norm(x, g, b):
    xf = x.astype(jnp.float32)
    mu = jnp.mean(xf, axis=-1, keepdims=True)
    var = jnp.mean(jnp.square(xf - mu), axis=-1, keepdims=True)
    y = (xf - mu) * lax.rsqrt(var + EPS)
    return (y * g.astype(jnp.float32) + b.astype(jnp.float32)).astype(x.dtype)


def pool_mixer(a, pool_scale, w_pool):
    B, S, _ = a.shape
    af = a.astype(jnp.float32)
    csum = jnp.cumsum(af, axis=1)
    pos1 = jnp.arange(1, S + 1, dtype=jnp.float32)
    groups = []
    for gi, w in enumerate(POOL_WINDOWS):
        sl = slice(gi * POOL_GROUP_WIDTH, (gi + 1) * POOL_GROUP_WIDTH)
        cg = csum[..., sl]
        prev = jnp.pad(cg, ((0, 0), (w, 0), (0, 0)))[:, :S]
        cnt = jnp.minimum(pos1, float(w))[None, :, None]
        groups.append((cg - prev) / cnt - af[..., sl])
    pooled = jnp.concatenate(groups, axis=-1) * pool_scale.astype(jnp.float32)
    pooled = pooled.astype(a.dtype).reshape(B, S, N_POOL_GROUPS, POOL_GROUP_WIDTH)
    y = jnp.einsum('bsgc,gcd->bsgd', pooled, w_pool)
    return y.reshape(B, S, D_MODEL)


def sgu_mixer(u, v, ln_v_g, ln_v_b, w_s, b_s, w_gproj):
    B, S, _ = u.shape
    u = jax.nn.gelu(u)
    v = layer_norm(jax.nn.gelu(v), ln_v_g, ln_v_b)
    nc = S // CHUNK
    v = v.reshape(B, nc, CHUNK, GMLP_HEADS, GMLP_HEAD_DIM)
    mask = jnp.tril(jnp.ones((CHUNK, CHUNK), dtype=w_s.dtype))
    sv = jnp.einsum('hts,bnshd->bnthd', w_s * mask[None], v)
    sv = sv + jnp.transpose(b_s)[None, None, :, :, None]
    y = u.reshape(B, nc, CHUNK, GMLP_HEADS, GMLP_HEAD_DIM) * sv
    return y.reshape(B, S, GMLP_WIDTH) @ w_gproj


def peer_ffn(h, w_q, sub_keys, expert_u, expert_v):
    B, S, D = h.shape
    q = (h @ w_q).astype(jnp.float32).reshape(B, S, PEER_HEADS, 2, HALF_DIM)
    kf = sub_keys.astype(jnp.float32)
    s1 = jnp.einsum('bshk,hnk->bshn', q[..., 0, :], kf[:, 0])
    s2 = jnp.einsum('bshk,hnk->bshn', q[..., 1, :], kf[:, 1])
    t1, i1 = lax.top_k(s1, TOPK)
    t2, i2 = lax.top_k(s2, TOPK)
    cand = (t1[..., :, None] + t2[..., None, :]).reshape(B, S, PEER_HEADS, TOPK * TOPK)
    cand_idx = (i1[..., :, None] * N_KEYS + i2[..., None, :]).reshape(B, S, PEER_HEADS, TOPK * TOPK)
    ts, ti = lax.top_k(cand, TOPK)
    idx = jnp.take_along_axis(cand_idx, ti, axis=-1)
    gate = jax.nn.softmax(ts, axis=-1)
    T = B * S
    E = PEER_HEADS * TOPK
    nb = T // TOKEN_BLOCK
    xb = h.reshape(nb, TOKEN_BLOCK, D)
    ib = idx.reshape(nb, TOKEN_BLOCK, E)
    gb = gate.reshape(nb, TOKEN_BLOCK, E).astype(h.dtype)

    def block(args):
        xt, it, gt = args
        ue = expert_u[it]
        ve = expert_v[it]
        act = jax.nn.gelu(jnp.einsum('td,ted->te', xt, ue))
        return jnp.einsum('te,ted->td', gt * act, ve)

    y = lax.map(block, (xb, ib, gb))
    return y.reshape(B, S, D)


def setup_inputs(seed: int = 0) -> dict:
    key = jax.random.key(seed)
    ks = jax.random.split(key, 20)
    f32 = jnp.float32
    nrm = lambda k, shape, s: jax.random.normal(k, shape, f32) * s
    x = nrm(ks[0], (BATCH, SEQ, D_MODEL), 1.0)
    norm1_g = 1.0 + nrm(ks[1], (D_MODEL,), 0.02)
    w_in = nrm(ks[2], (D_MODEL, IN_WIDTH), D_MODEL ** -0.5)
    b_gate = nrm(ks[3], (2 * D_MODEL,), 0.02)
    pool_scale = 1.0 + nrm(ks[4], (POOL_WIDTH,), 0.02)
    w_pool = nrm(ks[5], (N_POOL_GROUPS, POOL_GROUP_WIDTH, POOL_OUT_GROUP), POOL_GROUP_WIDTH ** -0.5)
    ln_v_g = 1.0 + nrm(ks[6], (GMLP_WIDTH,), 0.02)
    ln_v_b = nrm(ks[7], (GMLP_WIDTH,), 0.02)
    w_s = nrm(ks[8], (GMLP_HEADS, CHUNK, CHUNK), 0.5 * CHUNK ** -0.5)
    b_s = 1.0 + nrm(ks[9], (GMLP_HEADS, CHUNK), 0.02)
    w_gproj = nrm(ks[10], (GMLP_WIDTH, D_MODEL), GMLP_WIDTH ** -0.5)
    w_out = nrm(ks[11], (D_MODEL, D_MODEL), D_MODEL ** -0.5)
    norm2_g = 1.0 + nrm(ks[12], (D_MODEL,), 0.02)
    w_q = nrm(ks[13], (D_MODEL, PEER_HEADS * QUERY_DIM), D_MODEL ** -0.5)
    sub_keys = nrm(ks[14], (PEER_HEADS, 2, N_KEYS, HALF_DIM), HALF_DIM ** -0.5)
    expert_u = nrm(ks[15], (N_EXPERTS, D_MODEL), D_MODEL ** -0.5)
    expert_v = nrm(ks[16], (N_EXPERTS, D_MODEL), PEER_HEADS ** -0.5)
    final_g = 1.0 + nrm(ks[17], (D_MODEL,), 0.02)
    return {"x": x, "norm1_g": norm1_g, "w_in": w_in, "b_gate": b_gate,
            "pool_scale": pool_scale, "w_pool": w_pool, "ln_v_g": ln_v_g, "ln_v_b": ln_v_b,
            "w_s": w_s, "b_s": b_s, "w_gproj": w_gproj, "w_out": w_out,
            "norm2_g": norm2_g, "w_q": w_q, "sub_keys": sub_keys,
            "expert_u": expert_u, "expert_v": expert_v, "final_g": final_g}


def reference(x, norm1_g, w_in, b_gate, pool_scale, w_pool, ln_v_g, ln_v_b,
              w_s, b_s, w_gproj, w_out, norm2_g, w_q, sub_keys,
              expert_u, expert_v, final_g):
    P, G, D = POOL_WIDTH, GMLP_WIDTH, D_MODEL
    for _ in range(DEPTH):
        h = rms_norm(x, norm1_g)
        z = h @ w_in
        y_pool = pool_mixer(z[..., :P], pool_scale, w_pool)
        y_sgu = sgu_mixer(z[..., P:P + G], z[..., P + G:P + 2 * G],
                          ln_v_g, ln_v_b, w_s, b_s, w_gproj)
        gates = jax.nn.sigmoid(z[..., P + 2 * G:] + b_gate)
        merged = gates[..., :D] * y_pool + gates[..., D:] * y_sgu
        x = x + merged @ w_out
        x = x + peer_ffn(rms_norm(x, norm2_g), w_q, sub_keys, expert_u, expert_v)
    return rms_norm(x, final_g)
```

```python
from contextlib import ExitStack

import numpy as np
import concourse.bass as bass
import concourse.mybir as mybir
from concourse.bass_utils import run_bass_kernel_spmd

F32 = mybir.dt.float32
BF16 = mybir.dt.bfloat16
U32 = mybir.dt.uint32
AF = mybir.ActivationFunctionType
ALU = mybir.AluOpType
AX = mybir.AxisListType

NCORES = 8
D = 2048
KC = 16
T = 1024
HALO = 16
TH = T + HALO
EPS = 1e-6
NEXP = 16384
NEB = 128
S1_PER_TT = 16

ENGS = ("sync", "scalar", "vector", "gpsimd", "tensor")

C_G1, C_G2, C_GF, C_BG, C_PS, C_EPS = 0, 16, 32, 48, 80, 88
NCOLS = 89


class Op:
    __slots__ = ("eng", "fn", "deps", "signal", "dma", "sigval", "waits", "dma_idx")

    def __init__(self, eng, fn, dma):
        self.eng = eng
        self.fn = fn
        self.deps = []
        self.signal = False
        self.dma = dma
        self.sigval = None
        self.waits = []
        self.dma_idx = None


NS_DMA = 6


class Prog:
    def __init__(self):
        self.ops = {e: [] for e in ENGS}
        self.last_w = {}
        self.readers = {}
        self.barrier_deps = []
        self.last_dma = {}
        self.dma_ops = {}
        self.dma_eng = {}

    def op(self, eng, fn, reads=(), writes=(), dma=None):
        o = Op(eng, fn, dma)
        deps = []
        for r in reads:
            w = self.last_w.get(r)
            if w is not None:
                deps.append((w, "raw"))
        for w_ in writes:
            w = self.last_w.get(w_)
            if w is not None:
                deps.append((w, "waw"))
            for rd in self.readers.get(w_, ()):
                deps.append((rd, "war"))
        for r in reads:
            self.readers.setdefault(r, []).append(o)
        for w_ in writes:
            self.last_w[w_] = o
            self.readers[w_] = []
        seen = set()
        for d, kind in deps:
            if d is o or id(d) in seen:
                continue
            if d.dma is None and dma is None and d.eng == eng:
                if eng == "tensor":
                    continue
            seen.add(id(d))
            o.deps.append(d)
            d.signal = True
        for d in self.barrier_deps:
            if id(d) not in seen:
                seen.add(id(d))
                o.deps.append(d)
                d.signal = True
        if dma is not None:
            o.signal = True
            lst = self.dma_ops.setdefault(dma, [])
            assert self.dma_eng.setdefault(dma, eng) == eng, "one issuing engine per DMA channel"
            o.dma_idx = len(lst)
            if o.dma_idx >= NS_DMA:
                prev = lst[o.dma_idx - NS_DMA]
                if id(prev) not in seen:
                    seen.add(id(prev))
                    o.deps.append(prev)
            lst.append(o)
            self.last_dma[(dma, o.dma_idx % NS_DMA)] = o
        self.ops[eng].append(o)
        return o

    def barrier(self):
        bd = []
        for e in ENGS:
            for o in reversed(self.ops[e]):
                if o.dma is None and o.fn is not None:
                    bd.append(o)
                    break
        bd.extend(self.last_dma.values())
        self.barrier_deps = bd
        self.last_w = {}
        self.readers = {}

    def emit(self, sems, block):
        dma_count = {}
        for e in ENGS:
            c = 0
            for o in self.ops[e]:
                if o.dma is not None:
                    o.sigval = ("dma:%s:%d" % (o.dma, o.dma_idx % NS_DMA), 16 * (o.dma_idx // NS_DMA + 1))
                elif o.signal and o.fn is not None:
                    c += 1
                    o.sigval = (e, c)
        for e in ENGS:
            known = {}
            for o in self.ops[e]:
                need = {}
                for d in o.deps:
                    if d.sigval is None:
                        continue
                    s, v = d.sigval
                    if known.get(s, 0) >= v:
                        continue
                    if need.get(s, 0) < v:
                        need[s] = v
                for s, v in need.items():
                    known[s] = v
                o.waits = list(need.items())

        def mk(e):
            def body(engobj):
                for o in self.ops[e]:
                    for s, v in o.waits:
                        engobj.wait_ge(sems[s], v)
                    if o.fn is None:
                        continue
                    ins = o.fn(engobj)
                    if o.sigval is not None:
                        s, v = o.sigval
                        ins.then_inc(sems[s], 16 if s.startswith("dma:") else 1)
            return body

        for e in ENGS:
            if self.ops[e]:
                getattr(block, e)(mk(e))


_ESZ = {F32: 4, BF16: 2, U32: 4}


class Arena:
    def __init__(self, nc, es, name, nbytes):
        self.nbytes = nbytes
        self.t32 = es.enter_context(nc.sbuf_tensor(name, [128, nbytes // 4], F32))
        self.h = {F32: self.t32, BF16: self.t32.bitcast(BF16), U32: self.t32.bitcast(U32)}

    def view(self, byte_off, shape, dt):
        esz = _ESZ[dt]
        assert byte_off % 4 == 0
        n = 1
        for s in shape[1:]:
            n *= s
        assert byte_off + n * esz <= self.nbytes, (byte_off, shape, self.nbytes)
        rowlen = self.nbytes // esz
        dims = []
        stride = 1
        for s in reversed(shape[1:]):
            dims.append([stride, s])
            stride *= s
        dims.append([rowlen, shape[0]])
        return bass.AP(self.h[dt], byte_off // esz, list(reversed(dims)))


def ap_of(base, extra_off, dims):
    p = base.ap[0]
    return bass.AP(base.tensor, base.offset + extra_off, [list(p)] + [list(d) for d in dims])


_SHAPE_OVERRIDE = {}


class _Stop(Exception):
    pass


def build_program(dbg=(), upto=99):
    nc = bass.Bass("TRN2", target_bir_lowering=False)
    P = Prog()

    def stage(k):
        if k > upto:
            raise _Stop()

    def din(name, shape, dt=F32):
        shape = _SHAPE_OVERRIDE.get(name, shape)
        return nc.dram_tensor(name, shape, dt, kind="ExternalInput").ap()

    xT_d = din("xT", [D, TH])
    w_in_d = din("w_in", [D, 7168])
    w_out_d = din("w_out", [D, D])
    w_q_d = din("w_q", [D, D])
    w_gp_d = din("w_gproj", [1024, D])
    w_pool_d = din("w_pool", [4, 256, 512])
    wsT_d = din("wsT", [128, 1024])
    maskT_d = din("maskT", [128, 1024])
    bs_d = din("bs_row", [1, 1024])
    cols_d = din("cols", [128, NCOLS])
    lng_d = din("lng", [1, 1024])
    lnb_d = din("lnb", [1, 1024])
    invc_d = din("invc", [1, 64])
    keysT_d = din("keysT", [128, 2048])
    UT_d = din("UT", [D, NEXP])
    V_d = din("V", [NEXP, D])
    ident_d = din("ident", [128, 128])
    ones_d = din("ones", [128, 128])
    iota_d = din("iota", [128, 128])
    out_d = nc.dram_tensor("outT", [D, T], F32, kind="ExternalOutput").ap()
    Gd = nc.dram_tensor("Gd", [128, 128, T], BF16, kind=("ExternalOutput" if "Gd" in dbg else "Internal")).ap()
    Wd = nc.dram_tensor("Wd", [NEB, 128, T], BF16, kind=("ExternalOutput" if "Wd" in dbg else "Internal")).ap()
    Ad = nc.dram_tensor("Ad", [NEB, 128, T], BF16).ap()
    X2d = nc.dram_tensor("X2d", [D, T], F32).ap()
    dbg_out = {}

    with ExitStack() as es:
        HTA = Arena(nc, es, "HTA", KC * TH * 2)
        BIG = Arena(nc, es, "BIG", 80 * 1024)
        X2A = Arena(nc, es, "X2A", 64 * 1024)
        WGA = Arena(nc, es, "WGA", 16 * 1024)
        CST = Arena(nc, es, "CST", 11 * 1024)
        banks = [es.enter_context(nc.psum_tensor(f"bank{i}", [128, 512], F32)) for i in range(8)]
        sems = {e: es.enter_context(nc.semaphore("s_" + e)) for e in ENGS}
        for ch in ("ld", "wc", "gw", "gr", "ww", "wr", "w2", "out"):
            for k in range(NS_DMA):
                sems["dma:%s:%d" % (ch, k)] = es.enter_context(nc.semaphore("d_%s%d" % (ch, k)))
        block = es.enter_context(nc.Block())

        HT = HTA.view(0, [128, KC, TH], BF16)
        WB_OFF = 0
        M_OFF = 48 * 1024

        def WB(slot):
            if slot == 3:
                return WGA.view(0, [128, KC, 512], BF16)
            return BIG.view(WB_OFF + slot * 16384, [128, KC, 512], BF16)

        co = [0]

        def calloc(shape, dt):
            n = 1
            for s in shape[1:]:
                n *= s
            nb = (n * _ESZ[dt] + 31) // 32 * 32
            v = CST.view(co[0], shape, dt)
            co[0] += nb
            return v

        cols = calloc([128, NCOLS], F32)
        ident = calloc([128, 128], F32)
        ones_f = calloc([128, 128], F32)
        iota_f = calloc([128, 128], F32)
        WmT = calloc([128, 1024], BF16)
        keysT = calloc([128, 16, 128], BF16)
        invc = calloc([128, 64], F32)
        bs_row = calloc([1, 1024], BF16)
        ones_row = calloc([1, 128], BF16)
        iota_b = calloc([128, 128], BF16)

        pbc = [0]

        def nbank():
            pbc[0] = (pbc[0] + 1) % 8
            return pbc[0]

        def bcast_rows(d_ap, n):
            return bass.AP(d_ap.tensor, d_ap.offset, [[0, 128], [1, n]])

        def dump(name, ap_sb, shape, rkeys):
            if name not in dbg:
                return
            dt = ap_sb.dtype
            o = nc.dram_tensor("dbg_" + name, shape, dt, kind="ExternalOutput").ap()
            dbg_out[name] = o
            P.op("sync", lambda e: e.dma_start(out=o, in_=ap_sb), reads=rkeys, writes=[("dbg", name)], dma="out")

        try:
            stage(0)
            P.op("sync", lambda e: e.dma_start(out=cols, in_=cols_d), writes=["cols"], dma="ld")
            P.op("sync", lambda e: e.dma_start(out=ident, in_=ident_d), writes=["ident"], dma="ld")
            P.op("sync", lambda e: e.dma_start(out=ones_f, in_=ones_d), writes=["ones"], dma="ld")
            P.op("sync", lambda e: e.dma_start(out=iota_f, in_=iota_d), writes=["iota"], dma="ld")
            P.op("sync", lambda e: e.dma_start(out=invc, in_=bcast_rows(invc_d, 64)), writes=["invc"], dma="ld")
            P.op("gpsimd", lambda e: e.dma_start(out=keysT, in_=keysT_d.rearrange("p (a n) -> p a n", a=16)),
                 writes=["keysT"], dma="wc")
            P.op("gpsimd", lambda e: e.dma_start(out=bs_row, in_=bs_d), writes=["bs_row"], dma="wc")
            P.op("gpsimd", lambda e: e.memset(ones_row, 1.0), writes=["ones_row"])
            P.op("gpsimd", lambda e: e.dma_start(out=WB(3), in_=w_in_d.rearrange("(kc p) n -> p kc n", p=128)[:, :, 0:512]),
                 writes=[("wb", 3)], dma="wc")
            P.op("vector", lambda e: e.tensor_copy(out=iota_b, in_=iota_f), reads=["iota"], writes=["iota_b"])
            XS = BIG.view(0, [128, KC, TH], F32)
            tmpw = X2A.view(0, [128, 1024], F32)
            tmpm = X2A.view(4096, [128, 1024], F32)
            P.op("sync", lambda e: e.dma_start(out=tmpw, in_=wsT_d), writes=["tmpw"], dma="ld")
            P.op("sync", lambda e: e.dma_start(out=tmpm, in_=maskT_d), writes=["tmpm"], dma="ld")
            P.op("vector", lambda e: e.tensor_tensor(out=WmT, in0=tmpw, in1=tmpm, op=ALU.mult),
                 reads=["tmpw", "tmpm"], writes=["WmT"])

            stage(1)
            xT_v = xT_d.rearrange("(kc p) t -> p kc t", p=128)
            for q in range(4):
                P.op("sync", lambda e, q=q: e.dma_start(out=XS[:, 4 * q:4 * q + 4, :], in_=xT_v[:, 4 * q:4 * q + 4, :]),
                     writes=[("xs", q)], dma="ld")

            def rmsnorm(src_of_kc, src_keys_of_kc, ncols, gcol, out_of_kc, out_keys_of_kc, sq_views, rstd, tag):
                tiles = [(c0, min(c0 + 512, ncols)) for c0 in range(0, ncols, 512)]
                tb = [nbank() for _ in tiles]
                for kc in range(KC):
                    sq = sq_views[kc % len(sq_views)]
                    P.op("scalar", lambda e, kc=kc, sq=sq: e.activation(out=sq[:, 0:ncols], in_=src_of_kc(kc), func=AF.Square),
                         reads=src_keys_of_kc(kc), writes=[(tag + "sq", kc % len(sq_views))])
                    for (c0, c1), b in zip(tiles, tb):
                        P.op("tensor", lambda e, kc=kc, sq=sq, c0=c0, c1=c1, b=b: e.matmul(
                            banks[b][:, 0:c1 - c0], lhsT=ones_f, rhs=sq[:, c0:c1], start=(kc == 0), stop=(kc == KC - 1)),
                            reads=[(tag + "sq", kc % len(sq_views)), "ones"], writes=[("bank", b)])
                for (c0, c1), b in zip(tiles, tb):
                    P.op("scalar", lambda e, c0=c0, c1=c1, b=b: e.activation(
                        out=rstd[:, c0:c1], in_=banks[b][:, 0:c1 - c0], func=AF.Sqrt,
                        bias=cols[:, C_EPS:C_EPS + 1], scale=1.0 / D),
                        reads=[("bank", b), "cols"], writes=[(tag + "rs", c0)])
                    P.op("vector", lambda e, c0=c0, c1=c1: e.reciprocal(out=rstd[:, c0:c1], in_=rstd[:, c0:c1]),
                         reads=[(tag + "rs", c0)], writes=[(tag + "rstd", c0)])
                for kc in range(KC):
                    P.op("vector", lambda e, kc=kc: e.scalar_tensor_tensor(
                        out=out_of_kc(kc), in0=src_of_kc(kc), scalar=cols[:, gcol + kc:gcol + kc + 1],
                        in1=rstd[:, 0:ncols], op0=ALU.mult, op1=ALU.mult),
                        reads=src_keys_of_kc(kc) + [(tag + "rstd", c0) for (c0, _) in tiles] + ["cols"],
                        writes=out_keys_of_kc(kc))

            sq1 = [X2A.view(16384 + i * 4352, [128, TH], F32) for i in range(3)]
            rstd1 = X2A.view(32768, [128, TH], F32)
            rmsnorm(lambda kc: XS[:, kc, :], lambda kc: [("xs", kc // 4)], TH, C_G1,
                    lambda kc: HT[:, kc, :], lambda kc: [("ht", kc)], sq1, rstd1, "n1")
            dump("hT", HT, [128, KC, TH], [("ht", kc) for kc in range(KC)])
            P.barrier()

            w_in_v = w_in_d.rearrange("(kc p) n -> p kc n", p=128)
            w_out_v = w_out_d.rearrange("(kc p) n -> p kc n", p=128)
            w_q_v = w_q_d.rearrange("(kc p) n -> p kc n", p=128)
            groups = [w_in_v[:, :, g * 512:(g + 1) * 512] for g in range(6)]
            for qd in range(4):
                groups.append(w_in_v[:, :, 3072 + qd * 512:3072 + (qd + 1) * 512])
                groups.append(w_in_v[:, :, 5120 + qd * 512:5120 + (qd + 1) * 512])
            groups += [w_out_v[:, :, g * 512:(g + 1) * 512] for g in range(4)]
            groups += [w_q_v[:, :, g * 512:(g + 1) * 512] for g in range(4)]
            st_issued = [1]

            def st_get(i, ahead=2):
                while st_issued[0] <= min(i + ahead, len(groups) - 1):
                    n = st_issued[0]
                    P.op("gpsimd", lambda e, n=n: e.dma_start(out=WB(n % 3), in_=groups[n]),
                         writes=[("wb", n % 3)], dma="wc")
                    st_issued[0] += 1
                return 3 if i == 0 else i % 3

            stage(2)
            pooledT = X2A.view(0, [128, 8, T], BF16)
            yT = X2A.view(16384, [128, 8, T], BF16)
            TMP = 32768
            a_sb = [X2A.view(TMP + i * 4352, [128, TH], F32) for i in range(2)]
            pscr = [X2A.view(TMP + 8704 + i * 4352, [128, TH], F32) for i in range(2)]
            pd = X2A.view(TMP + 17408, [128, T], F32)
            pt16 = X2A.view(TMP + 21504, [128, 16], F32)
            a_tiles = [(0, 512), (512, 1024), (1024, TH)]
            for i_ in range(2):
                P.op("vector", lambda e, i_=i_: e.memset(pscr[i_], 0.0), writes=[("pscr", i_)])
            for grp in range(2):
                slot = st_get(grp)
                for nb in range(4):
                    cch = grp * 4 + nb
                    r = cch % 2
                    for (c0, c1) in a_tiles:
                        b = nbank()
                        for kc in range(KC):
                            P.op("tensor", lambda e, b=b, slot=slot, kc=kc, nb=nb, c0=c0, c1=c1: e.matmul(
                                banks[b][:, 0:c1 - c0], lhsT=WB(slot)[:, kc, nb * 128:(nb + 1) * 128],
                                rhs=HT[:, kc, c0:c1], start=(kc == 0), stop=(kc == KC - 1)),
                                reads=[("wb", slot), ("ht", kc)], writes=[("bank", b)])
                        P.op("scalar", lambda e, b=b, r=r, c0=c0, c1=c1: e.copy(out=a_sb[r][:, c0:c1], in_=banks[b][:, 0:c1 - c0]),
                             reads=[("bank", b)], writes=[("a_sb", r, c0)])
                    g = cch // 2
                    w = 2 << g
                    cur, curk = a_sb[r], [("a_sb", r, c0) for (c0, _) in a_tiles]
                    for k in range(g + 1):
                        sh = 1 << k
                        nxt = pscr[k % 2]
                        P.op("vector", lambda e, cur=cur, nxt=nxt, sh=sh: e.tensor_tensor(
                            out=nxt[:, sh:TH], in0=cur[:, sh:TH], in1=cur[:, 0:TH - sh], op=ALU.add),
                            reads=curk, writes=[("pscr", k % 2)])
                        cur, curk = nxt, [("pscr", k % 2)]
                    P.op("vector", lambda e, cur=cur, r=r, w=w: e.scalar_tensor_tensor(
                        out=pd, in0=cur[:, HALO:TH], scalar=1.0 / w, in1=a_sb[r][:, HALO:TH],
                        op0=ALU.mult, op1=ALU.subtract),
                        reads=curk + [("a_sb", r, c0) for (c0, _) in a_tiles], writes=["pd"])
                    P.op("vector", lambda e, cur=cur, g=g: e.tensor_tensor(
                        out=pt16, in0=cur[:, HALO:HALO + 16], in1=invc[:, g * 16:(g + 1) * 16], op=ALU.mult),
                        reads=curk + ["invc"], writes=["pt16"])
                    P.op("vector", lambda e, r=r: e.tensor_tensor(
                        out=pd[:, 0:16], in0=pt16, in1=a_sb[r][:, HALO:HALO + 16], op=ALU.subtract),
                        reads=["pt16", ("a_sb", r, 0), "pd"], writes=["pd"])
                    P.op("vector", lambda e, cch=cch: e.tensor_scalar(
                        out=pooledT[:, cch, :], in0=pd, scalar1=cols[:, C_PS + cch:C_PS + cch + 1], scalar2=None,
                        op0=ALU.mult),
                        reads=["pd", "cols"], writes=[("pooled", cch)])
            dump("pooledT", pooledT, [128, 8, T], [("pooled", c) for c in range(8)])

            stage(3)
            guT = BIG.view(M_OFF, [128, 8, T], F32)
            for grp in range(2, 4):
                slot = st_get(grp)
                for nb in range(4):
                    uc = (grp - 2) * 4 + nb
                    for th in range(2):
                        b = nbank()
                        for kc in range(KC):
                            P.op("tensor", lambda e, b=b, slot=slot, kc=kc, nb=nb, th=th: e.matmul(
                                banks[b][:, :], lhsT=WB(slot)[:, kc, nb * 128:(nb + 1) * 128],
                                rhs=HT[:, kc, HALO + th * 512:HALO + (th + 1) * 512], start=(kc == 0), stop=(kc == KC - 1)),
                                reads=[("wb", slot), ("ht", kc)], writes=[("bank", b)])
                        P.op("scalar", lambda e, b=b, uc=uc, th=th: e.activation(
                            out=guT[:, uc, th * 512:(th + 1) * 512], in_=banks[b][:, :], func=AF.Gelu_apprx_tanh),
                            reads=[("bank", b)], writes=[("gu", uc, th)])

            stage(4)
            P.barrier()
            gv = [X2A.view(TMP + i * 4096, [128, 1024], F32) for i in range(2)]
            v_tm = [X2A.view(TMP + 8192 + i * 2048, [128, 1024], BF16) for i in range(2)]
            lng_bc = X2A.view(TMP + 12288, [128, 1024], F32)
            lnb_bc = X2A.view(TMP + 16384, [128, 1024], F32)
            bst = X2A.view(TMP + 20480, [128, 2, 6], F32)
            mv = X2A.view(TMP + 20480 + 64, [128, 2], F32)
            rsv = X2A.view(TMP + 20480 + 96, [128, 1], F32)
            P.op("sync", lambda e: e.dma_start(out=lng_bc, in_=bcast_rows(lng_d, 1024)), writes=["lng"], dma="ld")
            P.op("sync", lambda e: e.dma_start(out=lnb_bc, in_=bcast_rows(lnb_d, 1024)), writes=["lnb"], dma="ld")
            sv = [st_get(4, ahead=1), st_get(5, ahead=1)]
            def v_front(tt):
                r = tt % 2
                for vc in range(2):
                    b = nbank()
                    for kc in range(KC):
                        P.op("tensor", lambda e, b=b, vc=vc, kc=kc, tt=tt: e.matmul(
                            banks[b][:, :], lhsT=HT[:, kc, HALO + tt * 128:HALO + (tt + 1) * 128],
                            rhs=WB(sv[vc])[:, kc, :], start=(kc == 0), stop=(kc == KC - 1)),
                            reads=[("wb", sv[vc]), ("ht", kc)], writes=[("bank", b)])
                    P.op("scalar", lambda e, b=b, r=r, vc=vc: e.activation(
                        out=gv[r][:, vc * 512:(vc + 1) * 512], in_=banks[b][:, :], func=AF.Gelu_apprx_tanh),
                        reads=[("bank", b)], writes=[("gv", r, vc), ("gvn", r), ("gvg", r)])
                    P.op("vector", lambda e, r=r, vc=vc: e.bn_stats(out=bst[:, vc, :], in_=gv[r][:, vc * 512:(vc + 1) * 512]),
                         reads=[("gv", r, vc)], writes=[("bst", vc)])
                P.op("vector", lambda e: e.bn_aggr(out=mv, in_=bst.rearrange("p a b -> p (a b)")), reads=[("bst", 0), ("bst", 1)], writes=["mv"])
                P.op("scalar", lambda e: e.activation(out=rsv, in_=mv[:, 1:2], func=AF.Sqrt,
                                                      bias=cols[:, C_EPS:C_EPS + 1], scale=1.0),
                     reads=["mv", "cols"], writes=["rsv0", "rsv"])
                P.op("vector", lambda e: e.reciprocal(out=rsv, in_=rsv), reads=["rsv0"], writes=["rsv"])
                P.op("vector", lambda e, r=r: e.tensor_scalar(
                    out=gv[r], in0=gv[r], scalar1=mv[:, 0:1], scalar2=rsv[:, 0:1], op0=ALU.subtract, op1=ALU.mult),
                    reads=[("gv", r, 0), ("gv", r, 1), "mv", "rsv"], writes=[("gvn", r)])
                P.op("vector", lambda e, r=r: e.tensor_tensor(out=gv[r], in0=gv[r], in1=lng_bc, op=ALU.mult),
                     reads=[("gvn", r), "lng"], writes=[("gvg", r)])
                P.op("vector", lambda e, r=r: e.tensor_tensor(out=v_tm[r], in0=gv[r], in1=lnb_bc, op=ALU.add),
                     reads=[("gvg", r), "lnb"], writes=[("vtm", r)])

            def v_back(tt):
                r = tt % 2
                for hq in range(2):
                    b = nbank()
                    for h4 in range(4):
                        h = hq * 4 + h4
                        P.op("tensor", lambda e, b=b, h=h, h4=h4, r=r: e.matmul(
                            banks[b][:, h4 * 128:(h4 + 1) * 128], lhsT=v_tm[r][:, h * 128:(h + 1) * 128],
                            rhs=WmT[:, h * 128:(h + 1) * 128], start=True, stop=False),
                            reads=[("vtm", r), "WmT"], writes=[("bank", b)])
                        P.op("tensor", lambda e, b=b, h=h, h4=h4: e.matmul(
                            banks[b][:, h4 * 128:(h4 + 1) * 128], lhsT=ones_row[0:1, :],
                            rhs=bs_row[0:1, h * 128:(h + 1) * 128], start=False, stop=True),
                            reads=["ones_row", "bs_row"], writes=[("bank", b)])
                    P.op("vector", lambda e, b=b, hq=hq, tt=tt: e.tensor_tensor(
                        out=yT[:, hq * 4:(hq + 1) * 4, tt * 128:(tt + 1) * 128],
                        in0=banks[b][:, :].rearrange("p (a t) -> p a t", a=4),
                        in1=guT[:, hq * 4:(hq + 1) * 4, tt * 128:(tt + 1) * 128], op=ALU.mult),
                        reads=[("bank", b)] + [("gu", hq * 4 + a, tt // 4) for a in range(4)],
                        writes=[("yT", hq, tt)])

            v_front(0)
            for tt in range(8):
                if tt + 1 < 8:
                    v_front(tt + 1)
                v_back(tt)
            dump("yT", yT, [128, 8, T], [("yT", hq, tt) for hq in range(2) for tt in range(8)])
            P.barrier()

            stage(5)
            mergedT = BIG.view(M_OFF, [128, KC, T], BF16)
            WP = WGA.view(0, [128, 4, 2, 512], BF16)
            WG = [WGA.view(8192 + i * 4096, [128, 8, 256], BF16) for i in range(2)]
            m1 = X2A.view(TMP, [128, 4, T], F32)
            sg = [X2A.view(TMP + 16384 + i * 2048, [128, 512], F32) for i in range(3)]
            t2 = [X2A.view(TMP + 22528 + i * 2048, [128, 512], F32) for i in range(2)]
            P.op("gpsimd", lambda e: e.dma_start(out=WP, in_=w_pool_d.rearrange("g (c p) d -> p g c d", p=128)),
                 writes=["WP"], dma="wc")
            w_gp_v = w_gp_d.rearrange("(kc p) n -> p kc n", p=128)
            sgc = [0]
            for quad in range(4):
                for half in range(2):
                    P.op("gpsimd", lambda e, quad=quad, half=half: e.dma_start(
                        out=WG[half], in_=w_gp_v[:, :, quad * 512 + half * 256:quad * 512 + (half + 1) * 256]),
                        writes=[("wg", half)], dma="wc")
                sA = st_get(6 + 2 * quad)
                for dc4 in range(4):
                    dc = quad * 4 + dc4
                    for th in range(2):
                        bga = nbank()
                        for kc in range(KC):
                            P.op("tensor", lambda e, b=bga, kc=kc, dc4=dc4, th=th, sA=sA: e.matmul(
                                banks[b][:, :], lhsT=WB(sA)[:, kc, dc4 * 128:(dc4 + 1) * 128],
                                rhs=HT[:, kc, HALO + th * 512:HALO + (th + 1) * 512], start=(kc == 0), stop=(kc == KC - 1)),
                                reads=[("wb", sA), ("ht", kc)], writes=[("bank", bga)])
                        byp = nbank()
                        for cc in range(2):
                            P.op("tensor", lambda e, b=byp, cc=cc, quad=quad, dc4=dc4, th=th: e.matmul(
                                banks[b][:, :], lhsT=WP[:, quad, cc, dc4 * 128:(dc4 + 1) * 128],
                                rhs=pooledT[:, 2 * quad + cc, th * 512:(th + 1) * 512], start=(cc == 0), stop=(cc == 1)),
                                reads=["WP", ("pooled", 2 * quad + cc)], writes=[("bank", byp)])
                        si = sgc[0] % 3
                        sgc[0] += 1
                        P.op("scalar", lambda e, b=bga, si=si, dc=dc: e.activation(
                            out=sg[si], in_=banks[b][:, :], func=AF.Sigmoid, bias=cols[:, C_BG + dc:C_BG + dc + 1], scale=1.0),
                            reads=[("bank", bga), "cols"], writes=[("sg", si)])
                        P.op("vector", lambda e, b=byp, si=si, dc4=dc4, th=th: e.tensor_tensor(
                            out=m1[:, dc4, th * 512:(th + 1) * 512], in0=banks[b][:, :], in1=sg[si], op=ALU.mult),
                            reads=[("bank", byp), ("sg", si)], writes=[("m1", dc4, th)])
                sB = st_get(7 + 2 * quad)
                for half in range(2):
                    wgi = half
                    for d2 in range(2):
                        dc4 = half * 2 + d2
                        dc = quad * 4 + dc4
                        for th in range(2):
                            bgb = nbank()
                            for kc in range(KC):
                                P.op("tensor", lambda e, b=bgb, kc=kc, dc4=dc4, th=th, sB=sB: e.matmul(
                                    banks[b][:, :], lhsT=WB(sB)[:, kc, dc4 * 128:(dc4 + 1) * 128],
                                    rhs=HT[:, kc, HALO + th * 512:HALO + (th + 1) * 512], start=(kc == 0), stop=(kc == KC - 1)),
                                    reads=[("wb", sB), ("ht", kc)], writes=[("bank", bgb)])
                            bys = nbank()
                            for kc in range(8):
                                P.op("tensor", lambda e, b=bys, kc=kc, d2=d2, th=th, wgi=wgi: e.matmul(
                                    banks[b][:, :], lhsT=WG[wgi][:, kc, d2 * 128:(d2 + 1) * 128],
                                    rhs=yT[:, kc, th * 512:(th + 1) * 512], start=(kc == 0), stop=(kc == 7)),
                                    reads=[("wg", wgi), "yTall"], writes=[("bank", bys)])
                            si = sgc[0] % 3
                            sgc[0] += 1
                            ti = sgc[0] % 2
                            P.op("scalar", lambda e, b=bgb, si=si, dc=dc: e.activation(
                                out=sg[si], in_=banks[b][:, :], func=AF.Sigmoid,
                                bias=cols[:, C_BG + 16 + dc:C_BG + 16 + dc + 1], scale=1.0),
                                reads=[("bank", bgb), "cols"], writes=[("sg", si)])
                            P.op("vector", lambda e, b=bys, si=si, ti=ti: e.tensor_tensor(
                                out=t2[ti], in0=banks[b][:, :], in1=sg[si], op=ALU.mult),
                                reads=[("bank", bys), ("sg", si)], writes=[("t2", ti)])
                            P.op("vector", lambda e, ti=ti, dc=dc, dc4=dc4, th=th: e.tensor_tensor(
                                out=mergedT[:, dc, th * 512:(th + 1) * 512], in0=t2[ti],
                                in1=m1[:, dc4, th * 512:(th + 1) * 512], op=ALU.add),
                                reads=[("t2", ti), ("m1", dc4, th)], writes=[("merged", dc, th)])
            dump("mergedT", mergedT, [128, KC, T], [("merged", dc, th) for dc in range(KC) for th in range(2)])
            dump("m1", m1, [128, 4, T], [])
            dump("yT5", yT, [128, 8, T], [])
            dump("pooledT5", pooledT, [128, 8, T], [])
            dump("t2a", t2[0], [128, 512], [])
            dump("t2b", t2[1], [128, 512], [])
            dump("sga", sg[0], [128, 512], [])
            dump("sgb", sg[1], [128, 512], [])
            dump("sgc", sg[2], [128, 512], [])
            P.barrier()

            stage(6)
            x2T = X2A.view(0, [128, KC, T], F32)
            for q in range(4):
                P.op("sync", lambda e, q=q: e.dma_start(out=x2T[:, 4 * q:4 * q + 4, :], in_=xT_v[:, 4 * q:4 * q + 4, HALO:TH]),
                     writes=[("x2", 4 * q + i, th) for i in range(4) for th in range(2)], dma="ld")
            for grp in range(4):
                slot = st_get(14 + grp)
                for nb in range(4):
                    dc = grp * 4 + nb
                    for th in range(2):
                        b = nbank()
                        for kc in range(KC):
                            P.op("tensor", lambda e, b=b, slot=slot, kc=kc, nb=nb, th=th: e.matmul(
                                banks[b][:, :], lhsT=WB(slot)[:, kc, nb * 128:(nb + 1) * 128],
                                rhs=mergedT[:, kc, th * 512:(th + 1) * 512], start=(kc == 0), stop=(kc == KC - 1)),
                                reads=[("wb", slot), "mergedall"], writes=[("bank", b)])
                        P.op("vector", lambda e, b=b, dc=dc, th=th: e.tensor_tensor(
                            out=x2T[:, dc, th * 512:(th + 1) * 512], in0=banks[b][:, :],
                            in1=x2T[:, dc, th * 512:(th + 1) * 512], op=ALU.add),
                            reads=[("bank", b), ("x2", dc, th)], writes=[("x2", dc, th)])
            dump("x2T", x2T, [128, KC, T], [("x2", dc, th) for dc in range(KC) for th in range(2)])
            P.barrier()

            stage(7)
            sq2 = [WGA.view(i * 4096, [128, T], F32) for i in range(2)]
            rstd2 = WGA.view(8192, [128, T], F32)
            rmsnorm(lambda kc: x2T[:, kc, :], lambda kc: [], T, C_G2,
                    lambda kc: HT[:, kc, HALO:TH], lambda kc: [("ht", kc)], sq2, rstd2, "n2")
            X2d_v = X2d.rearrange("(kc p) t -> p kc t", p=128)
            for q in range(4):
                P.op("sync", lambda e, q=q: e.dma_start(out=X2d_v[:, 4 * q:4 * q + 4, :], in_=x2T[:, 4 * q:4 * q + 4, :]),
                     writes=[("x2d", q)], dma="out")

            stage(8)
            qT = BIG.view(M_OFF, [128, KC, T], BF16)
            UT_v = UT_d.rearrange("(kc p) e -> p kc e", p=128)
            for grp in range(4):
                slot = st_get(18 + grp)
                if grp == 3:
                    P.op("gpsimd", lambda e: e.dma_start(out=WB(1), in_=UT_v[:, :, 0:512]), writes=[("wb", 1)], dma="wc")
                for nb in range(4):
                    qc = grp * 4 + nb
                    for th in range(2):
                        b = nbank()
                        for kc in range(KC):
                            P.op("tensor", lambda e, b=b, slot=slot, kc=kc, nb=nb, th=th: e.matmul(
                                banks[b][:, :], lhsT=WB(slot)[:, kc, nb * 128:(nb + 1) * 128],
                                rhs=HT[:, kc, HALO + th * 512:HALO + (th + 1) * 512], start=(kc == 0), stop=(kc == KC - 1)),
                                reads=[("wb", slot), ("ht", kc)], writes=[("bank", b)])
                        P.op("scalar", lambda e, b=b, qc=qc, th=th: e.copy(
                            out=qT[:, qc, th * 512:(th + 1) * 512], in_=banks[b][:, :]),
                            reads=[("bank", b)], writes=[("qT", qc, th)])
            P.barrier()

            stage(9)
            def xv(off, shape, dt):
                return X2A.view(off, shape, dt)
            s_sb = [xv(0, [128, 16, 128], F32)]
            s_scr = xv(8192, [128, 16, 128], F32)
            cand = xv(8192, [128, 8, 256], F32)
            oh = xv(16384, [128, 8, 16, 16], F32)
            t16 = xv(24576, [128, 16, 16], F32)
            i16 = xv(25600, [128, 16, 16], U32)
            i16f = xv(26624, [128, 16, 16], F32)
            cscr = [xv(27648 + i * 1024, [128, 256], F32) for i in range(2)]
            ts = xv(29696, [128, 8, 16], F32)
            pos = xv(30208, [128, 8, 16], U32)
            posf = xv(30720, [128, 8, 16], F32)
            af_ = xv(31232, [128, 8, 16], F32)
            bf_ = xv(31744, [128, 8, 16], F32)
            posa_u = xv(32256 - 512 - 512 + 0, [128, 8, 16], U32) if False else WGA.view(13824, [128, 8, 16], U32)
            posb_u = WGA.view(14336, [128, 8, 16], U32)
            zs = WGA.view(14848, [128, 8], F32)
            sel = [WGA.view(12288 + i * 512, [128, 128], F32) for i in range(3)]
            ijgT = WGA.view(0, [128, 3, T], F32)
            Ub = [BIG.view(i * 16384, [128, KC, 512], BF16) for i in range(2)]
            Ado = [BIG.view(32768 + i * 4096, [128, 2, T], BF16) for i in range(2)]
            NSR = 16
            SR = [(BIG.view(40960 + i * 256, [128, 128], BF16), BIG.view(40960 + 4096 + i * 256, [128, 128], BF16))
                  for i in range(NSR)]
            Gs = [X2A.view(32768, [128, 128, 128], BF16), X2A.view(0, [128, 128, 128], BF16)]
            UT_v = UT_d.rearrange("(kc p) e -> p kc e", p=128)
            Ad_w = Ad.rearrange("b e t -> e b t")
            Gd_v = Gd.rearrange("i j t -> j i t")
            NG1 = NEB // 4

            def s1_load_u(gi):
                P.op("gpsimd", lambda e, gi=gi: e.dma_start(out=Ub[(gi + 1) % 2], in_=UT_v[:, :, gi * 512:(gi + 1) * 512]),
                     writes=[("Ub", (gi + 1) % 2)], dma="wc")

            def s1_chains():
                for gi in range(NG1):
                    if gi + 1 < NG1:
                        s1_load_u(gi + 1)
                    for eb4 in range(4):
                        pair = gi * 2 + eb4 // 2
                        slot = pair % 2
                        for th in range(2):
                            b = nbank()
                            for kc in range(KC):
                                P.op("tensor", lambda e, b=b, gi=gi, kc=kc, eb4=eb4, th=th: e.matmul(
                                    banks[b][:, :], lhsT=Ub[(gi + 1) % 2][:, kc, eb4 * 128:(eb4 + 1) * 128],
                                    rhs=HT[:, kc, HALO + th * 512:HALO + (th + 1) * 512],
                                    start=(kc == 0), stop=(kc == KC - 1)),
                                    reads=[("Ub", (gi + 1) % 2)], writes=[("bank", b)])
                            P.op("scalar", lambda e, b=b, slot=slot, eb4=eb4, th=th: e.activation(
                                out=Ado[slot][:, eb4 % 2, th * 512:(th + 1) * 512], in_=banks[b][:, :],
                                func=AF.Gelu_apprx_tanh),
                                reads=[("bank", b)], writes=[("Ado", slot, eb4 % 2, th)])
                            if eb4 % 2 == 1 and th == 1:
                                P.op("sync", lambda e, slot=slot, pair=pair: e.dma_start(
                                    out=Ad_w[:, pair * 2:pair * 2 + 2, :], in_=Ado[slot]),
                                    reads=[("Ado", slot, a, c) for a in range(2) for c in range(2)],
                                    writes=[("Ad", pair)], dma="ww")
                            yield

            s1_it = s1_chains()

            def s1_step(n):
                for _ in range(n):
                    if next(s1_it, "done") == "done":
                        return

            def transposes(tt):
                bt = nbank()
                for w3 in range(3):
                    P.op("tensor", lambda e, w3=w3, bt=bt: e.transpose(out=banks[bt][:, w3 * 128:(w3 + 1) * 128], in_=sel[w3],
                                                                       identity=ident),
                         reads=[("sel", w3), "ident"], writes=[("bank", bt)])
                P.op("scalar", lambda e, bt=bt, tt=tt: e.copy(
                    out=ijgT[:, :, tt * 128:(tt + 1) * 128], in_=banks[bt][:, 0:384].rearrange("p (a t) -> p a t", a=3)),
                    reads=[("bank", bt)], writes=[("ijgT", tt)])

            prev_cand_readers = [(k_, h_) for k_ in ("tsa", "tsb", "posa", "posb") for h_ in range(8)]
            for tt in range(8):
                r = 0
                sb_ = [nbank() for _ in range(4)]
                for hh in range(16):
                    b = sb_[hh // 4]
                    P.op("tensor", lambda e, b=b, hh=hh, tt=tt: e.matmul(
                        banks[b][:, (hh % 4) * 128:(hh % 4 + 1) * 128], lhsT=qT[:, hh, tt * 128:(tt + 1) * 128],
                        rhs=keysT[:, hh, :], start=True, stop=True),
                        reads=["keysT", "qTall"], writes=[("bank", b)])
                for q4 in range(4):
                    P.op("scalar", lambda e, q4=q4, r=r, b=sb_[q4]: e.copy(
                        out=s_sb[r][:, q4 * 4:(q4 + 1) * 4, :], in_=banks[b][:, :].rearrange("p (a n) -> p a n", a=4)),
                        reads=[("bank", sb_[q4])], writes=[("s_sb", r, q4)])
                if tt > 0:
                    transposes(tt - 1)
                for hh in range(16):
                    P.op("vector", lambda e, hh=hh, r=r: e.max(out=t16[:, hh, 0:8], in_=s_sb[r][:, hh, :]),
                         reads=[("s_sb", r, hh // 4)], writes=[("t16a", hh)])
                for hh in range(16):
                    P.op("vector", lambda e, hh=hh, r=r: e.max_index(out=i16[:, hh, 0:8], in_max=t16[:, hh, 0:8],
                                                                     in_values=s_sb[r][:, hh, :]),
                         reads=[("s_sb", r, hh // 4), ("t16a", hh)], writes=[("i16a", hh)])
                for hh in range(16):
                    P.op("vector", lambda e, hh=hh, r=r: e.match_replace(out=s_scr[:, hh, :], in_to_replace=t16[:, hh, 0:8],
                                                                         in_values=s_sb[r][:, hh, :], imm_value=-1e30),
                         reads=[("s_sb", r, hh // 4), ("t16a", hh), "cand"] + prev_cand_readers, writes=[("s_scr", hh)])
                for hh in range(16):
                    P.op("vector", lambda e, hh=hh: e.max(out=t16[:, hh, 8:16], in_=s_scr[:, hh, :]),
                         reads=[("s_scr", hh)], writes=[("t16b", hh)])
                for hh in range(16):
                    P.op("vector", lambda e, hh=hh: e.max_index(out=i16[:, hh, 8:16], in_max=t16[:, hh, 8:16],
                                                                in_values=s_scr[:, hh, :]),
                         reads=[("s_scr", hh), ("t16b", hh)], writes=[("i16b", hh)])
                allt = [("t16a", hh) for hh in range(16)] + [("t16b", hh) for hh in range(16)]
                alli = [("i16a", hh) for hh in range(16)] + [("i16b", hh) for hh in range(16)]
                P.op("vector", lambda e: e.tensor_copy(out=i16f, in_=i16), reads=alli, writes=["i16f"])
                P.op("vector", lambda e: e.tensor_tensor(
                    out=cand.rearrange("p h (a b) -> p h a b", a=16),
                    in0=ap_of(t16, 0, [[32, 8], [1, 16], [0, 16]]),
                    in1=ap_of(t16, 16, [[32, 8], [0, 16], [1, 16]]), op=ALU.add),
                    reads=allt + alli + [("s_scr", hh) for hh in range(16)], writes=["cand"])
                for h in range(8):
                    P.op("vector", lambda e, h=h: e.max(out=ts[:, h, 0:8], in_=cand[:, h, :]),
                         reads=["cand"], writes=[("tsa", h)])
                for h in range(8):
                    P.op("vector", lambda e, h=h: e.max_index(out=pos[:, h, 0:8], in_max=ts[:, h, 0:8], in_values=cand[:, h, :]),
                         reads=["cand", ("tsa", h)], writes=[("posa", h)])
                for h in range(8):
                    c_ = cscr[h % 2]
                    P.op("vector", lambda e, h=h, c_=c_: e.match_replace(out=c_, in_to_replace=ts[:, h, 0:8],
                                                                         in_values=cand[:, h, :], imm_value=-1e30),
                         reads=["cand", ("tsa", h)], writes=[("cscr", h % 2)])
                    P.op("vector", lambda e, h=h, c_=c_: e.max(out=ts[:, h, 8:16], in_=c_),
                         reads=[("cscr", h % 2)], writes=[("tsb", h)])
                    P.op("vector", lambda e, h=h, c_=c_: e.max_index(out=pos[:, h, 8:16], in_max=ts[:, h, 8:16], in_values=c_),
                         reads=[("cscr", h % 2), ("tsb", h)], writes=[("posb", h)])
                allts = [("tsa", h) for h in range(8)] + [("tsb", h) for h in range(8)]
                allpos = [("posa", h) for h in range(8)] + [("posb", h) for h in range(8)]
                P.op("vector", lambda e: e.tensor_single_scalar(out=posa_u, in_=pos, scalar=4, op=ALU.logical_shift_right),
                     reads=allpos, writes=["pa_u"])
                P.op("vector", lambda e: e.tensor_single_scalar(out=posb_u, in_=pos, scalar=15, op=ALU.bitwise_and),
                     reads=allpos, writes=["pb_u"])
                P.op("vector", lambda e: e.tensor_copy(out=af_, in_=posa_u), reads=["pa_u"], writes=["af"])
                P.op("vector", lambda e: e.tensor_copy(out=bf_, in_=posb_u), reads=["pb_u"], writes=["bf"])
                for which, src, off, dst in ((0, af_, 0, sel[0]), (1, bf_, 16, sel[1])):
                    P.op("vector", lambda e, src=src: e.tensor_tensor(
                        out=oh, in0=ap_of(src, 0, [[16, 8], [1, 16], [0, 16]]),
                        in1=ap_of(iota_f, 0, [[0, 8], [0, 16], [1, 16]]), op=ALU.is_equal),
                        reads=["af", "bf", "iota"], writes=["oh0", "oh1"])
                    P.op("vector", lambda e, off=off: e.tensor_tensor(
                        out=oh, in0=oh, in1=ap_of(i16f, off, [[32, 8], [0, 16], [1, 16]]), op=ALU.mult),
                        reads=["oh0", "i16f"], writes=["oh1"])
                    P.op("vector", lambda e, dst=dst: e.tensor_reduce(
                        out=dst.rearrange("p (h k) -> p h k", h=8), in_=oh, axis=AX.X, op=ALU.add),
                        reads=["oh1"], writes=[("sel", which)])
                P.op("vector", lambda e: e.tensor_tensor(
                    out=posf, in0=ts, in1=ap_of(ts, 0, [[16, 8], [0, 16]]), op=ALU.subtract),
                    reads=allts, writes=["tsd", "tse"])
                s1_step(S1_PER_TT - 4)
                P.op("scalar", lambda e: e.activation(out=posf, in_=posf, func=AF.Exp), reads=["tsd"], writes=["tse"])
                P.op("vector", lambda e: e.tensor_reduce(out=zs, in_=posf, axis=AX.X, op=ALU.add),
                     reads=["tse"], writes=["zs0", "zs"])
                P.op("vector", lambda e: e.reciprocal(out=zs, in_=zs), reads=["zs0"], writes=["zs"])
                P.op("vector", lambda e: e.tensor_tensor(
                    out=sel[2].rearrange("p (h k) -> p h k", h=8), in0=posf, in1=ap_of(zs, 0, [[1, 8], [0, 16]]), op=ALU.mult),
                    reads=["tse", "zs"], writes=[("sel", 2)])
                s1_step(4)
            transposes(7)
            Vr0 = BIG.view(M_OFF, [128, 8, D], BF16)
            V_v0 = V_d.rearrange("(b e) d -> e b d", e=128)
            for hf in range(2):
                P.op("gpsimd", lambda e, hf=hf: e.dma_start(
                    out=Vr0[:, hf * 4:(hf + 1) * 4, :], in_=V_v0[:, hf * 4:(hf + 1) * 4, :]),
                    writes=["qTall", ("Vr", 0, hf)], dma="wc")
            dump("ijgT", ijgT, [128, 3, T], [("ijgT", tt) for tt in range(8)])
            tokc = [0]
            for tt in range(8):
                g = tt % 2
                for q in range(32):
                    b = nbank()
                    for t4 in range(4):
                        tok = q * 4 + t4
                        t = tt * 128 + tok
                        si = tokc[0] % NSR
                        tokc[0] += 1
                        S_, R_ = SR[si]
                        P.op("vector", lambda e, S_=S_, t=t: e.tensor_scalar(
                            out=S_, in0=iota_b, scalar1=ijgT[:, 1, t:t + 1], scalar2=None, op0=ALU.is_equal),
                            reads=["iota_b", ("ijgT", tt)], writes=[("S", si)])
                        P.op("vector", lambda e, R_=R_, t=t: e.tensor_scalar(
                            out=R_, in0=iota_b, scalar1=ijgT[:, 0, t:t + 1], scalar2=ijgT[:, 2, t:t + 1],
                            op0=ALU.is_equal, op1=ALU.mult),
                            reads=["iota_b", ("ijgT", tt)], writes=[("R", si)])
                        P.op("tensor", lambda e, b=b, t4=t4, S_=S_, R_=R_: e.matmul(
                            banks[b][:, t4 * 128:(t4 + 1) * 128], lhsT=S_, rhs=R_, start=True, stop=True),
                            reads=[("S", si), ("R", si)], writes=[("bank", b)])
                    P.op("scalar", lambda e, b=b, g=g, q=q: e.copy(
                        out=Gs[g][:, :, q * 4:(q + 1) * 4],
                        in_=ap_of(banks[b][:, :], 0, [[1, 128], [128, 4]])),
                        reads=[("bank", b)], writes=[("Gs", g, q)])
                    if q % 2 == 1 or q in (6, 22):
                        s1_step(1)
                for i8 in range(8):
                    P.op("sync", lambda e, g=g, i8=i8, tt=tt: e.dma_start(
                        out=Gd_v[:, i8 * 16:(i8 + 1) * 16, tt * 128:(tt + 1) * 128], in_=Gs[g][:, i8 * 16:(i8 + 1) * 16, :]),
                        reads=[("Gs", g, q) for q in range(32)], writes=[("Gd", tt, i8)], dma="gw")
            s1_step(10 ** 6)
            P.barrier()

            stage(12)
            Vr = [BIG.view(M_OFF, [128, 8, D], BF16), BIG.view(0, [128, 8, D], BF16)]
            Ar = [BIG.view(32768, [128, 8, T], BF16), HTA.view(0, [128, 8, T], BF16)]
            Gr = [HTA.view(16384, [128, 8, T], BF16), WGA.view(0, [128, 8, T], BF16)]
            V_v = V_d.rearrange("(b e) d -> e b d", e=128)
            Ad_r = Ad.rearrange("b e t -> e b t")
            Gd_r = Gd.rearrange("i j t -> j i t")
            NG2 = NEB // 8

            def s2_load(gi, extra_reads=()):
                s_ = gi % 2
                for hf in range(2 if gi > 0 else 0):
                    P.op("gpsimd", lambda e, s_=s_, gi=gi, hf=hf: e.dma_start(
                        out=Vr[s_][:, hf * 4:(hf + 1) * 4, :], in_=V_v[:, gi * 8 + hf * 4:gi * 8 + (hf + 1) * 4, :]),
                        reads=list(extra_reads), writes=[("Vr", s_, hf)], dma="wc")
                P.op("sync", lambda e, s_=s_, gi=gi: e.dma_start(out=Ar[s_], in_=Ad_r[:, gi * 8:(gi + 1) * 8, :]),
                     reads=list(extra_reads), writes=[("Ar", s_, 0), ("Ar", s_, 1), ("W", s_, 0), ("W", s_, 1)], dma="wr")
                P.op("sync", lambda e, s_=s_, gi=gi: e.dma_start(out=Gr[s_], in_=Gd_r[:, gi * 8:(gi + 1) * 8, :]),
                     reads=list(extra_reads), writes=[("Gr", s_)], dma="gr")

            def s2_mult(gi):
                s_ = gi % 2
                for hf in range(2):
                    P.op("vector", lambda e, s_=s_, hf=hf: e.tensor_tensor(
                        out=Ar[s_][:, hf * 4:(hf + 1) * 4, :], in0=Ar[s_][:, hf * 4:(hf + 1) * 4, :],
                        in1=Gr[s_][:, hf * 4:(hf + 1) * 4, :], op=ALU.mult),
                        reads=[("Ar", s_, hf), ("Gr", s_)], writes=[("Ar", s_, hf), ("W", s_, hf)])

            s2_load(0)
            P.op("sync", lambda e: e.dma_start(out=x2T[:, 0:4, :], in_=X2d_v[:, 0:4, :]), writes=[("x2r", 0)], dma="ld")
            s2_mult(0)
            for q in range(1, 4):
                P.op("sync", lambda e, q=q: e.dma_start(out=x2T[:, 4 * q:4 * q + 4, :], in_=X2d_v[:, 4 * q:4 * q + 4, :]),
                     reads=[("W", 0, 0)], writes=[("x2r", q)], dma="ld")
            s2_load(1, extra_reads=[("W", 0, 0)])
            rnd = [0]
            for gi in range(NG2):
                s_ = gi % 2
                for r8 in range(8):
                    if r8 == 6 and gi + 1 < NG2:
                        s2_mult(gi + 1)
                    base = (rnd[0] % 2) * 4
                    rnd[0] += 1
                    for k in range(4):
                        dc = r8 * 2 + k // 2
                        th = k % 2
                        b = base + k
                        for e8 in range(8):
                            P.op("tensor", lambda e, b=b, s_=s_, e8=e8, dc=dc, th=th: e.matmul(
                                banks[b][:, :], lhsT=Vr[s_][:, e8, dc * 128:(dc + 1) * 128],
                                rhs=Ar[s_][:, e8, th * 512:(th + 1) * 512], start=(e8 == 0), stop=(e8 == 7)),
                                reads=[("Vr", s_, e8 // 4), ("W", s_, e8 // 4)], writes=[("bank", b)])
                        P.op("vector", lambda e, b=b, dc=dc, th=th: e.tensor_tensor(
                            out=x2T[:, dc, th * 512:(th + 1) * 512], in0=banks[b][:, :],
                            in1=x2T[:, dc, th * 512:(th + 1) * 512], op=ALU.add),
                            reads=[("bank", b), ("x2r", dc // 4)], writes=[("x3", dc, th)])
                if gi + 2 < NG2:
                    s2_load(gi + 2)
            P.barrier()

            stage(13)
            sq3 = [WGA.view(i * 4096, [128, T], F32) for i in range(2)]
            rmsnorm(lambda kc: x2T[:, kc, :], lambda kc: [], T, C_GF,
                    lambda kc: x2T[:, kc, :], lambda kc: [("o", kc)], sq3, rstd2, "n3")
            out_v = out_d.rearrange("(kc p) t -> p kc t", p=128)
            for q in range(4):
                P.op("sync", lambda e, q=q: e.dma_start(out=out_v[:, 4 * q:4 * q + 4, :], in_=x2T[:, 4 * q:4 * q + 4, :]),
                     reads=[("o", 4 * q + i) for i in range(4)], writes=[("out", q)], dma="out")

        except _Stop:
            pass
        P.barrier()
        P.op("sync", None)
        P.emit(sems, block)
    return nc, list(dbg_out.keys())


def prepare_inputs(inputs, ncores=NCORES):
    f = np.float32
    x = np.asarray(inputs["x"], f)[0]
    xpad = np.concatenate([np.zeros((HALO, D), f), x], axis=0)
    w_s = np.asarray(inputs["w_s"], f)
    wsT = np.ascontiguousarray(w_s.transpose(2, 0, 1).reshape(128, 1024))
    s_idx = np.arange(128)[:, None]
    t_idx = np.arange(128)[None, :]
    maskT = np.tile((s_idx <= t_idx).astype(f), (1, 8))
    cols = np.zeros((128, NCOLS), f)
    cols[:, C_G1:C_G1 + 16] = np.asarray(inputs["norm1_g"], f).reshape(16, 128).T
    cols[:, C_G2:C_G2 + 16] = np.asarray(inputs["norm2_g"], f).reshape(16, 128).T
    cols[:, C_GF:C_GF + 16] = np.asarray(inputs["final_g"], f).reshape(16, 128).T
    cols[:, C_BG:C_BG + 32] = np.asarray(inputs["b_gate"], f).reshape(32, 128).T
    cols[:, C_PS:C_PS + 8] = np.asarray(inputs["pool_scale"], f).reshape(8, 128).T
    cols[:, C_EPS] = EPS
    keysT = np.ascontiguousarray(np.asarray(inputs["sub_keys"], f).reshape(16, 128, 128).transpose(2, 0, 1).reshape(128, 2048))
    UT = np.ascontiguousarray(np.asarray(inputs["expert_u"], f).T)
    shared = {
        "w_in": np.ascontiguousarray(inputs["w_in"], f),
        "w_out": np.ascontiguousarray(inputs["w_out"], f),
        "w_q": np.ascontiguousarray(inputs["w_q"], f),
        "w_gproj": np.ascontiguousarray(inputs["w_gproj"], f),
        "w_pool": np.ascontiguousarray(inputs["w_pool"], f),
        "wsT": wsT, "maskT": maskT,
        "bs_row": np.ascontiguousarray(np.asarray(inputs["b_s"], f).reshape(1, 1024)),
        "cols": cols,
        "lng": np.ascontiguousarray(np.asarray(inputs["ln_v_g"], f).reshape(1, 1024)),
        "lnb": np.ascontiguousarray(np.asarray(inputs["ln_v_b"], f).reshape(1, 1024)),
        "keysT": keysT, "UT": UT, "V": np.ascontiguousarray(inputs["expert_v"], f),
        "ident": np.eye(128, dtype=f), "ones": np.ones((128, 128), f),
        "iota": np.tile(np.arange(128, dtype=f)[None, :], (128, 1)),
    }
    in_maps = []
    for c in range(ncores):
        m = dict(shared)
        m["xT"] = np.ascontiguousarray(xpad[c * T:c * T + TH].T)
        pos1 = np.arange(c * T + 1, c * T + 17, dtype=f)
        m["invc"] = np.concatenate([1.0 / np.minimum(pos1, float(w)) for w in (2, 4, 8, 16)]).reshape(1, 64).astype(f)
        in_maps.append(m)
    return in_maps


_CACHE = {}


def kernel(**inputs):
    if "nc" not in _CACHE:
        _CACHE["nc"] = build_program()[0]
    nc = _CACHE["nc"]
    in_maps = prepare_inputs(inputs)
    res = run_bass_kernel_spmd(nc, in_maps, core_ids=list(range(NCORES)))
    outT = [np.asarray(r["outT"]) for r in res.results]
    out = np.concatenate([o.T for o in outT], axis=0)
    return np.ascontiguousarray(out.reshape(1, NCORES * T, D).astype(np.float32))
```

```python
from contextlib import ExitStack

import numpy as np
import concourse.bass as bass
import concourse.mybir as mybir
from concourse.bass_utils import run_bass_kernel_spmd

F32 = mybir.dt.float32
BF16 = mybir.dt.bfloat16
U32 = mybir.dt.uint32
AF = mybir.ActivationFunctionType
ALU = mybir.AluOpType
AX = mybir.AxisListType

NCORES = 8
D = 2048
KC = 16
T = 1024
HALO = 16
TH = T + HALO
EPS = 1e-6
NEXP = 16384
NEB = 128
S1_PER_TT = 16

ENGS = ("sync", "scalar", "vector", "gpsimd", "tensor")

C_G1, C_G2, C_GF, C_BG, C_PS, C_EPS = 0, 16, 32, 48, 80, 88
NCOLS = 89


class Op:
    __slots__ = ("eng", "fn", "deps", "signal", "dma", "sigval", "waits", "dma_idx")

    def __init__(self, eng, fn, dma):
        self.eng = eng
        self.fn = fn
        self.deps = []
        self.signal = False
        self.dma = dma
        self.sigval = None
        self.waits = []
        self.dma_idx = None


NS_DMA = 6


class Prog:
    def __init__(self):
        self.ops = {e: [] for e in ENGS}
        self.last_w = {}
        self.readers = {}
        self.barrier_deps = []
        self.last_dma = {}
        self.dma_ops = {}
        self.dma_eng = {}

    def op(self, eng, fn, reads=(), writes=(), dma=None):
        o = Op(eng, fn, dma)
        deps = []
        for r in reads:
            w = self.last_w.get(r)
            if w is not None:
                deps.append((w, "raw"))
        for w_ in writes:
            w = self.last_w.get(w_)
            if w is not None:
                deps.append((w, "waw"))
            for rd in self.readers.get(w_, ()):
                deps.append((rd, "war"))
        for r in reads:
            self.readers.setdefault(r, []).append(o)
        for w_ in writes:
            self.last_w[w_] = o
            self.readers[w_] = []
        seen = set()
        for d, kind in deps:
            if d is o or id(d) in seen:
                continue
            if d.dma is None and dma is None and d.eng == eng:
                if eng == "tensor":
                    continue
            seen.add(id(d))
            o.deps.append(d)
            d.signal = True
        for d in self.barrier_deps:
            if id(d) not in seen:
                seen.add(id(d))
                o.deps.append(d)
                d.signal = True
        if dma is not None:
            o.signal = True
            lst = self.dma_ops.setdefault(dma, [])
            assert self.dma_eng.setdefault(dma, eng) == eng, "one issuing engine per DMA channel"
            o.dma_idx = len(lst)
            if o.dma_idx >= NS_DMA:
                prev = lst[o.dma_idx - NS_DMA]
                if id(prev) not in seen:
                    seen.add(id(prev))
                    o.deps.append(prev)
            lst.append(o)
            self.last_dma[(dma, o.dma_idx % NS_DMA)] = o
        self.ops[eng].append(o)
        return o

    def barrier(self):
        bd = []
        for e in ENGS:
            for o in reversed(self.ops[e]):
                if o.dma is None and o.fn is not None:
                    bd.append(o)
                    break
        bd.extend(self.last_dma.values())
        self.barrier_deps = bd
        self.last_w = {}
        self.readers = {}

    def emit(self, sems, block):
        dma_count = {}
        for e in ENGS:
            c = 0
            for o in self.ops[e]:
                if o.dma is not None:
                    o.sigval = ("dma:%s:%d" % (o.dma, o.dma_idx % NS_DMA), 16 * (o.dma_idx // NS_DMA + 1))
                elif o.signal and o.fn is not None:
                    c += 1
                    o.sigval = (e, c)
        for e in ENGS:
            known = {}
            for o in self.ops[e]:
                need = {}
                for d in o.deps:
                    if d.sigval is None:
                        continue
                    s, v = d.sigval
                    if known.get(s, 0) >= v:
                        continue
                    if need.get(s, 0) < v:
                        need[s] = v
                for s, v in need.items():
                    known[s] = v
                o.waits = list(need.items())

        def mk(e):
            def body(engobj):
                for o in self.ops[e]:
                    for s, v in o.waits:
                        engobj.wait_ge(sems[s], v)
                    if o.fn is None:
                        continue
                    ins = o.fn(engobj)
                    if o.sigval is not None:
                        s, v = o.sigval
                        ins.then_inc(sems[s], 16 if s.startswith("dma:") else 1)
            return body

        for e in ENGS:
            if self.ops[e]:
                getattr(block, e)(mk(e))


_ESZ = {F32: 4, BF16: 2, U32: 4}


class Arena:
    def __init__(self, nc, es, name, nbytes):
        self.nbytes = nbytes
        self.t32 = es.enter_context(nc.sbuf_tensor(name, [128, nbytes // 4], F32))
        self.h = {F32: self.t32, BF16: self.t32.bitcast(BF16), U32: self.t32.bitcast(U32)}

    def view(self, byte_off, shape, dt):
        esz = _ESZ[dt]
        assert byte_off % 4 == 0
        n = 1
        for s in shape[1:]:
            n *= s
        assert byte_off + n * esz <= self.nbytes, (byte_off, shape, self.nbytes)
        rowlen = self.nbytes // esz
        dims = []
        stride = 1
        for s in reversed(shape[1:]):
            dims.append([stride, s])
            stride *= s
        dims.append([rowlen, shape[0]])
        return bass.AP(self.h[dt], byte_off // esz, list(reversed(dims)))


def ap_of(base, extra_off, dims):
    p = base.ap[0]
    return bass.AP(base.tensor, base.offset + extra_off, [list(p)] + [list(d) for d in dims])


_SHAPE_OVERRIDE = {}


class _Stop(Exception):
    pass


def build_program(dbg=(), upto=99):
    nc = bass.Bass("TRN2", target_bir_lowering=False)
    P = Prog()

    def stage(k):
        if k > upto:
            raise _Stop()

    def din(name, shape, dt=F32):
        shape = _SHAPE_OVERRIDE.get(name, shape)
        return nc.dram_tensor(name, shape, dt, kind="ExternalInput").ap()

    xT_d = din("xT", [D, TH])
    w_in_d = din("w_in", [D, 7168])
    w_out_d = din("w_out", [D, D])
    w_q_d = din("w_q", [D, D])
    w_gp_d = din("w_gproj", [1024, D])
    w_pool_d = din("w_pool", [4, 256, 512])
    wsT_d = din("wsT", [128, 1024])
    maskT_d = din("maskT", [128, 1024])
    bs_d = din("bs_row", [1, 1024])
    cols_d = din("cols", [128, NCOLS])
    lng_d = din("lng", [1, 1024])
    lnb_d = din("lnb", [1, 1024])
    invc_d = din("invc", [1, 64])
    keysT_d = din("keysT", [128, 2048])
    UT_d = din("UT", [D, NEXP])
    V_d = din("V", [NEXP, D])
    ident_d = din("ident", [128, 128])
    ones_d = din("ones", [128, 128])
    iota_d = din("iota", [128, 128])
    out_d = nc.dram_tensor("outT", [D, T], F32, kind="ExternalOutput").ap()
    Gd = nc.dram_tensor("Gd", [128, 128, T], BF16, kind=("ExternalOutput" if "Gd" in dbg else "Internal")).ap()
    Wd = nc.dram_tensor("Wd", [NEB, 128, T], BF16, kind=("ExternalOutput" if "Wd" in dbg else "Internal")).ap()
    Ad = nc.dram_tensor("Ad", [NEB, 128, T], BF16).ap()
    X2d = nc.dram_tensor("X2d", [D, T], F32).ap()
    dbg_out = {}

    with ExitStack() as es:
        HTA = Arena(nc, es, "HTA", KC * TH * 2)
        BIG = Arena(nc, es, "BIG", 80 * 1024)
        X2A = Arena(nc, es, "X2A", 64 * 1024)
        WGA = Arena(nc, es, "WGA", 16 * 1024)
        CST = Arena(nc, es, "CST", 11 * 1024)
        banks = [es.enter_context(nc.psum_tensor(f"bank{i}", [128, 512], F32)) for i in range(8)]
        sems = {e: es.enter_context(nc.semaphore("s_" + e)) for e in ENGS}
        for ch in ("ld", "wc", "gw", "gr", "ww", "wr", "w2", "out"):
            for k in range(NS_DMA):
                sems["dma:%s:%d" % (ch, k)] = es.enter_context(nc.semaphore("d_%s%d" % (ch, k)))
        block = es.enter_context(nc.Block())

        HT = HTA.view(0, [128, KC, TH], BF16)
        WB_OFF = 0
        M_OFF = 48 * 1024

        def WB(slot):
            if slot == 3:
                return WGA.view(0, [128, KC, 512], BF16)
            return BIG.view(WB_OFF + slot * 16384, [128, KC, 512], BF16)

        co = [0]

        def calloc(shape, dt):
            n = 1
            for s in shape[1:]:
                n *= s
            nb = (n * _ESZ[dt] + 31) // 32 * 32
            v = CST.view(co[0], shape, dt)
            co[0] += nb
            return v

        cols = calloc([128, NCOLS], F32)
        ident = calloc([128, 128], F32)
        ones_f = calloc([128, 128], F32)
        iota_f = calloc([128, 128], F32)
        WmT = calloc([128, 1024], BF16)
        keysT = calloc([128, 16, 128], BF16)
        invc = calloc([128, 64], F32)
        bs_row = calloc([1, 1024], BF16)
        ones_row = calloc([1, 128], BF16)
        iota_b = calloc([128, 128], BF16)

        pbc = [0]

        def nbank():
            pbc[0] = (pbc[0] + 1) % 8
            return pbc[0]

        def bcast_rows(d_ap, n):
            return bass.AP(d_ap.tensor, d_ap.offset, [[0, 128], [1, n]])

        def dump(name, ap_sb, shape, rkeys):
            if name not in dbg:
                return
            dt = ap_sb.dtype
            o = nc.dram_tensor("dbg_" + name, shape, dt, kind="ExternalOutput").ap()
            dbg_out[name] = o
            P.op("sync", lambda e: e.dma_start(out=o, in_=ap_sb), reads=rkeys, writes=[("dbg", name)], dma="out")

        try:
            stage(0)
            P.op("sync", lambda e: e.dma_start(out=cols, in_=cols_d), writes=["cols"], dma="ld")
            P.op("sync", lambda e: e.dma_start(out=ident, in_=ident_d), writes=["ident"], dma="ld")
            P.op("sync", lambda e: e.dma_start(out=ones_f, in_=ones_d), writes=["ones"], dma="ld")
            P.op("sync", lambda e: e.dma_start(out=iota_f, in_=iota_d), writes=["iota"], dma="ld")
            P.op("sync", lambda e: e.dma_start(out=invc, in_=bcast_rows(invc_d, 64)), writes=["invc"], dma="ld")
            P.op("gpsimd", lambda e: e.dma_start(out=keysT, in_=keysT_d.rearrange("p (a n) -> p a n", a=16)),
                 writes=["keysT"], dma="wc")
            P.op("gpsimd", lambda e: e.dma_start(out=bs_row, in_=bs_d), writes=["bs_row"], dma="wc")
            P.op("gpsimd", lambda e: e.memset(ones_row, 1.0), writes=["ones_row"])
            P.op("gpsimd", lambda e: e.dma_start(out=WB(3), in_=w_in_d.rearrange("(kc p) n -> p kc n", p=128)[:, :, 0:512]),
                 writes=[("wb", 3)], dma="wc")
            P.op("vector", lambda e: e.tensor_copy(out=iota_b, in_=iota_f), reads=["iota"], writes=["iota_b"])
            XS = BIG.view(0, [128, KC, TH], F32)
            tmpw = X2A.view(0, [128, 1024], F32)
            tmpm = X2A.view(4096, [128, 1024], F32)
            P.op("sync", lambda e: e.dma_start(out=tmpw, in_=wsT_d), writes=["tmpw"], dma="ld")
            P.op("sync", lambda e: e.dma_start(out=tmpm, in_=maskT_d), writes=["tmpm"], dma="ld")
            P.op("vector", lambda e: e.tensor_tensor(out=WmT, in0=tmpw, in1=tmpm, op=ALU.mult),
                 reads=["tmpw", "tmpm"], writes=["WmT"])

            stage(1)
            xT_v = xT_d.rearrange("(kc p) t -> p kc t", p=128)
            for q in range(4):
                P.op("sync", lambda e, q=q: e.dma_start(out=XS[:, 4 * q:4 * q + 4, :], in_=xT_v[:, 4 * q:4 * q + 4, :]),
                     writes=[("xs", q)], dma="ld")

            def rmsnorm(src_of_kc, src_keys_of_kc, ncols, gcol, out_of_kc, out_keys_of_kc, sq_views, rstd, tag):
                tiles = [(c0, min(c0 + 512, ncols)) for c0 in range(0, ncols, 512)]
                tb = [nbank() for _ in tiles]
                for kc in range(KC):
                    sq = sq_views[kc % len(sq_views)]
                    P.op("scalar", lambda e, kc=kc, sq=sq: e.activation(out=sq[:, 0:ncols], in_=src_of_kc(kc), func=AF.Square),
                         reads=src_keys_of_kc(kc), writes=[(tag + "sq", kc % len(sq_views))])
                    for (c0, c1), b in zip(tiles, tb):
                        P.op("tensor", lambda e, kc=kc, sq=sq, c0=c0, c1=c1, b=b: e.matmul(
                            banks[b][:, 0:c1 - c0], lhsT=ones_f, rhs=sq[:, c0:c1], start=(kc == 0), stop=(kc == KC - 1)),
                            reads=[(tag + "sq", kc % len(sq_views)), "ones"], writes=[("bank", b)])
                for (c0, c1), b in zip(tiles, tb):
                    P.op("scalar", lambda e, c0=c0, c1=c1, b=b: e.activation(
                        out=rstd[:, c0:c1], in_=banks[b][:, 0:c1 - c0], func=AF.Sqrt,
                        bias=cols[:, C_EPS:C_EPS + 1], scale=1.0 / D),
                        reads=[("bank", b), "cols"], writes=[(tag + "rs", c0)])
                    P.op("vector", lambda e, c0=c0, c1=c1: e.reciprocal(out=rstd[:, c0:c1], in_=rstd[:, c0:c1]),
                         reads=[(tag + "rs", c0)], writes=[(tag + "rstd", c0)])
                for kc in range(KC):
                    P.op("vector", lambda e, kc=kc: e.scalar_tensor_tensor(
                        out=out_of_kc(kc), in0=src_of_kc(kc), scalar=cols[:, gcol + kc:gcol + kc + 1],
                        in1=rstd[:, 0:ncols], op0=ALU.mult, op1=ALU.mult),
                        reads=src_keys_of_kc(kc) + [(tag + "rstd", c0) for (c0, _) in tiles] + ["cols"],
                        writes=out_keys_of_kc(kc))

            sq1 = [X2A.view(16384 + i * 4352, [128, TH], F32) for i in range(3)]
            rstd1 = X2A.view(32768, [128, TH], F32)
            rmsnorm(lambda kc: XS[:, kc, :], lambda kc: [("xs", kc // 4)], TH, C_G1,
                    lambda kc: HT[:, kc, :], lambda kc: [("ht", kc)], sq1, rstd1, "n1")
            dump("hT", HT, [128, KC, TH], [("ht", kc) for kc in range(KC)])
            P.barrier()

            w_in_v = w_in_d.rearrange("(kc p) n -> p kc n", p=128)
            w_out_v = w_out_d.rearrange("(kc p) n -> p kc n", p=128)
            w_q_v = w_q_d.rearrange("(kc p) n -> p kc n", p=128)
            groups = [w_in_v[:, :, g * 512:(g + 1) * 512] for g in range(6)]
            for qd in range(4):
                groups.append(w_in_v[:, :, 3072 + qd * 512:3072 + (qd + 1) * 512])
                groups.append(w_in_v[:, :, 5120 + qd * 512:5120 + (qd + 1) * 512])
            groups += [w_out_v[:, :, g * 512:(g + 1) * 512] for g in range(4)]
            groups += [w_q_v[:, :, g * 512:(g + 1) * 512] for g in range(4)]
            st_issued = [1]

            def st_get(i, ahead=2):
                while st_issued[0] <= min(i + ahead, len(groups) - 1):
                    n = st_issued[0]
                    P.op("gpsimd", lambda e, n=n: e.dma_start(out=WB(n % 3), in_=groups[n]),
                         writes=[("wb", n % 3)], dma="wc")
                    st_issued[0] += 1
                return 3 if i == 0 else i % 3

            stage(2)
            pooledT = X2A.view(0, [128, 8, T], BF16)
            yT = X2A.view(16384, [128, 8, T], BF16)
            TMP = 32768
            a_sb = [X2A.view(TMP + i * 4352, [128, TH], F32) for i in range(2)]
            pscr = [X2A.view(TMP + 8704 + i * 4352, [128, TH], F32) for i in range(2)]
            pd = X2A.view(TMP + 17408, [128, T], F32)
            pt16 = X2A.view(TMP + 21504, [128, 16], F32)
            a_tiles = [(0, 512), (512, 1024), (1024, TH)]
            for i_ in range(2):
                P.op("vector", lambda e, i_=i_: e.memset(pscr[i_], 0.0), writes=[("pscr", i_)])
            for grp in range(2):
                slot = st_get(grp)
                for nb in range(4):
                    cch = grp * 4 + nb
                    r = cch % 2
                    for (c0, c1) in a_tiles:
                        b = nbank()
                        for kc in range(KC):
                            P.op("tensor", lambda e, b=b, slot=slot, kc=kc, nb=nb, c0=c0, c1=c1: e.matmul(
                                banks[b][:, 0:c1 - c0], lhsT=WB(slot)[:, kc, nb * 128:(nb + 1) * 128],
                                rhs=HT[:, kc, c0:c1], start=(kc == 0), stop=(kc == KC - 1)),
                                reads=[("wb", slot), ("ht", kc)], writes=[("bank", b)])
                        P.op("scalar", lambda e, b=b, r=r, c0=c0, c1=c1: e.copy(out=a_sb[r][:, c0:c1], in_=banks[b][:, 0:c1 - c0]),
                             reads=[("bank", b)], writes=[("a_sb", r, c0)])
                    g = cch // 2
                    w = 2 << g
                    cur, curk = a_sb[r], [("a_sb", r, c0) for (c0, _) in a_tiles]
                    for k in range(g + 1):
                        sh = 1 << k
                        nxt = pscr[k % 2]
                        P.op("vector", lambda e, cur=cur, nxt=nxt, sh=sh: e.tensor_tensor(
                            out=nxt[:, sh:TH], in0=cur[:, sh:TH], in1=cur[:, 0:TH - sh], op=ALU.add),
                            reads=curk, writes=[("pscr", k % 2)])
                        cur, curk = nxt, [("pscr", k % 2)]
                    P.op("vector", lambda e, cur=cur, r=r, w=w: e.scalar_tensor_tensor(
                        out=pd, in0=cur[:, HALO:TH], scalar=1.0 / w, in1=a_sb[r][:, HALO:TH],
                        op0=ALU.mult, op1=ALU.subtract),
                        reads=curk + [("a_sb", r, c0) for (c0, _) in a_tiles], writes=["pd"])
                    P.op("vector", lambda e, cur=cur, g=g: e.tensor_tensor(
                        out=pt16, in0=cur[:, HALO:HALO + 16], in1=invc[:, g * 16:(g + 1) * 16], op=ALU.mult),
                        reads=curk + ["invc"], writes=["pt16"])
                    P.op("vector", lambda e, r=r: e.tensor_tensor(
                        out=pd[:, 0:16], in0=pt16, in1=a_sb[r][:, HALO:HALO + 16], op=ALU.subtract),
                        reads=["pt16", ("a_sb", r, 0), "pd"], writes=["pd"])
                    P.op("vector", lambda e, cch=cch: e.tensor_scalar(
                        out=pooledT[:, cch, :], in0=pd, scalar1=cols[:, C_PS + cch:C_PS + cch + 1], scalar2=None,
                        op0=ALU.mult),
                        reads=["pd", "cols"], writes=[("pooled", cch)])
            dump("pooledT", pooledT, [128, 8, T], [("pooled", c) for c in range(8)])

            stage(3)
            guT = BIG.view(M_OFF, [128, 8, T], F32)
            for grp in range(2, 4):
                slot = st_get(grp)
                for nb in range(4):
                    uc = (grp - 2) * 4 + nb
                    for th in range(2):
                        b = nbank()
                        for kc in range(KC):
                            P.op("tensor", lambda e, b=b, slot=slot, kc=kc, nb=nb, th=th: e.matmul(
                                banks[b][:, :], lhsT=WB(slot)[:, kc, nb * 128:(nb + 1) * 128],
                                rhs=HT[:, kc, HALO + th * 512:HALO + (th + 1) * 512], start=(kc == 0), stop=(kc == KC - 1)),
                                reads=[("wb", slot), ("ht", kc)], writes=[("bank", b)])
                        P.op("scalar", lambda e, b=b, uc=uc, th=th: e.activation(
                            out=guT[:, uc, th * 512:(th + 1) * 512], in_=banks[b][:, :], func=AF.Gelu_apprx_tanh),
                            reads=[("bank", b)], writes=[("gu", uc, th)])

            stage(4)
            P.barrier()
            gv = [X2A.view(TMP + i * 4096, [128, 1024], F32) for i in range(2)]
            v_tm = [X2A.view(TMP + 8192 + i * 2048, [128, 1024], BF16) for i in range(2)]
            lng_bc = X2A.view(TMP + 12288, [128, 1024], F32)
            lnb_bc = X2A.view(TMP + 16384, [128, 1024], F32)
            bst = X2A.view(TMP + 20480, [128, 2, 6], F32)
            mv = X2A.view(TMP + 20480 + 64, [128, 2], F32)
            rsv = X2A.view(TMP + 20480 + 96, [128, 1], F32)
            P.op("sync", lambda e: e.dma_start(out=lng_bc, in_=bcast_rows(lng_d, 1024)), writes=["lng"], dma="ld")
            P.op("sync", lambda e: e.dma_start(out=lnb_bc, in_=bcast_rows(lnb_d, 1024)), writes=["lnb"], dma="ld")
            sv = [st_get(4, ahead=1), st_get(5, ahead=1)]
            def v_front(tt):
                r = tt % 2
                for vc in range(2):
                    b = nbank()
                    for kc in range(KC):
                        P.op("tensor", lambda e, b=b, vc=vc, kc=kc, tt=tt: e.matmul(
                            banks[b][:, :], lhsT=HT[:, kc, HALO + tt * 128:HALO + (tt + 1) * 128],
                            rhs=WB(sv[vc])[:, kc, :], start=(kc == 0), stop=(kc == KC - 1)),
                            reads=[("wb", sv[vc]), ("ht", kc)], writes=[("bank", b)])
                    P.op("scalar", lambda e, b=b, r=r, vc=vc: e.activation(
                        out=gv[r][:, vc * 512:(vc + 1) * 512], in_=banks[b][:, :], func=AF.Gelu_apprx_tanh),
                        reads=[("bank", b)], writes=[("gv", r, vc), ("gvn", r), ("gvg", r)])
                    P.op("vector", lambda e, r=r, vc=vc: e.bn_stats(out=bst[:, vc, :], in_=gv[r][:, vc * 512:(vc + 1) * 512]),
                         reads=[("gv", r, vc)], writes=[("bst", vc)])
                P.op("vector", lambda e: e.bn_aggr(out=mv, in_=bst.rearrange("p a b -> p (a b)")), reads=[("bst", 0), ("bst", 1)], writes=["mv"])
                P.op("scalar", lambda e: e.activation(out=rsv, in_=mv[:, 1:2], func=AF.Sqrt,
                                                      bias=cols[:, C_EPS:C_EPS + 1], scale=1.0),
                     reads=["mv", "cols"], writes=["rsv0", "rsv"])
                P.op("vector", lambda e: e.reciprocal(out=rsv, in_=rsv), reads=["rsv0"], writes=["rsv"])
                P.op("vector", lambda e, r=r: e.tensor_scalar(
                    out=gv[r], in0=gv[r], scalar1=mv[:, 0:1], scalar2=rsv[:, 0:1], op0=ALU.subtract, op1=ALU.mult),
                    reads=[("gv", r, 0), ("gv", r, 1), "mv", "rsv"], writes=[("gvn", r)])
                P.op("vector", lambda e, r=r: e.tensor_tensor(out=gv[r], in0=gv[r], in1=lng_bc, op=ALU.mult),
                     reads=[("gvn", r), "lng"], writes=[("gvg", r)])
                P.op("vector", lambda e, r=r: e.tensor_tensor(out=v_tm[r], in0=gv[r], in1=lnb_bc, op=ALU.add),
                     reads=[("gvg", r), "lnb"], writes=[("vtm", r)])

            def v_back(tt):
                r = tt % 2
                for hq in range(2):
                    b = nbank()
                    for h4 in range(4):
                        h = hq * 4 + h4
                        P.op("tensor", lambda e, b=b, h=h, h4=h4, r=r: e.matmul(
                            banks[b][:, h4 * 128:(h4 + 1) * 128], lhsT=v_tm[r][:, h * 128:(h + 1) * 128],
                            rhs=WmT[:, h * 128:(h + 1) * 128], start=True, stop=False),
                            reads=[("vtm", r), "WmT"], writes=[("bank", b)])
                        P.op("tensor", lambda e, b=b, h=h, h4=h4: e.matmul(
                            banks[b][:, h4 * 128:(h4 + 1) * 128], lhsT=ones_row[0:1, :],
                            rhs=bs_row[0:1, h * 128:(h + 1) * 128], start=False, stop=True),
                            reads=["ones_row", "bs_row"], writes=[("bank", b)])
                    P.op("vector", lambda e, b=b, hq=hq, tt=tt: e.tensor_tensor(
                        out=yT[:, hq * 4:(hq + 1) * 4, tt * 128:(tt + 1) * 128],
                        in0=banks[b][:, :].rearrange("p (a t) -> p a t", a=4),
                        in1=guT[:, hq * 4:(hq + 1) * 4, tt * 128:(tt + 1) * 128], op=ALU.mult),
                        reads=[("bank", b)] + [("gu", hq * 4 + a, tt // 4) for a in range(4)],
                        writes=[("yT", hq, tt)])

            v_front(0)
            for tt in range(8):
                if tt + 1 < 8:
                    v_front(tt + 1)
                v_back(tt)
            dump("yT", yT, [128, 8, T], [("yT", hq, tt) for hq in range(2) for tt in range(8)])
            P.barrier()

            stage(5)
            mergedT = BIG.view(M_OFF, [128, KC, T], BF16)
            WP = WGA.view(0, [128, 4, 2, 512], BF16)
            WG = [WGA.view(8192 + i * 4096, [128, 8, 256], BF16) for i in range(2)]
            m1 = X2A.view(TMP, [128, 4, T], F32)
            sg = [X2A.view(TMP + 16384 + i * 2048, [128, 512], F32) for i in range(3)]
            t2 = [X2A.view(TMP + 22528 + i * 2048, [128, 512], F32) for i in range(2)]
            P.op("gpsimd", lambda e: e.dma_start(out=WP, in_=w_pool_d.rearrange("g (c p) d -> p g c d", p=128)),
                 writes=["WP"], dma="wc")
            w_gp_v = w_gp_d.rearrange("(kc p) n -> p kc n", p=128)
            sgc = [0]
            for quad in range(4):
                for half in range(2):
                    P.op("gpsimd", lambda e, quad=quad, half=half: e.dma_start(
                        out=WG[half], in_=w_gp_v[:, :, quad * 512 + half * 256:quad * 512 + (half + 1) * 256]),
                        writes=[("wg", half)], dma="wc")
                sA = st_get(6 + 2 * quad)
                for dc4 in range(4):
                    dc = quad * 4 + dc4
                    for th in range(2):
                        bga = nbank()
                        for kc in range(KC):
                            P.op("tensor", lambda e, b=bga, kc=kc, dc4=dc4, th=th, sA=sA: e.matmul(
                                banks[b][:, :], lhsT=WB(sA)[:, kc, dc4 * 128:(dc4 + 1) * 128],
                                rhs=HT[:, kc, HALO + th * 512:HALO + (th + 1) * 512], start=(kc == 0), stop=(kc == KC - 1)),
                                reads=[("wb", sA), ("ht", kc)], writes=[("bank", bga)])
                        byp = nbank()
                        for cc in range(2):
                            P.op("tensor", lambda e, b=byp, cc=cc, quad=quad, dc4=dc4, th=th: e.matmul(
                                banks[b][:, :], lhsT=WP[:, quad, cc, dc4 * 128:(dc4 + 1) * 128],
                                rhs=pooledT[:, 2 * quad + cc, th * 512:(th + 1) * 512], start=(cc == 0), stop=(cc == 1)),
                                reads=["WP", ("pooled", 2 * quad + cc)], writes=[("bank", byp)])
                        si = sgc[0] % 3
                        sgc[0] += 1
                        P.op("scalar", lambda e, b=bga, si=si, dc=dc: e.activation(
                            out=sg[si], in_=banks[b][:, :], func=AF.Sigmoid, bias=cols[:, C_BG + dc:C_BG + dc + 1], scale=1.0),
                            reads=[("bank", bga), "cols"], writes=[("sg", si)])
                        P.op("vector", lambda e, b=byp, si=si, dc4=dc4, th=th: e.tensor_tensor(
                            out=m1[:, dc4, th * 512:(th + 1) * 512], in0=banks[b][:, :], in1=sg[si], op=ALU.mult),
                            reads=[("bank", byp), ("sg", si)], writes=[("m1", dc4, th)])
                sB = st_get(7 + 2 * quad)
                for half in range(2):
                    wgi = half
                    for d2 in range(2):
                        dc4 = half * 2 + d2
                        dc = quad * 4 + dc4
                        for th in range(2):
                            bgb = nbank()
                            for kc in range(KC):
                                P.op("tensor", lambda e, b=bgb, kc=kc, dc4=dc4, th=th, sB=sB: e.matmul(
                                    banks[b][:, :], lhsT=WB(sB)[:, kc, dc4 * 128:(dc4 + 1) * 128],
                                    rhs=HT[:, kc, HALO + th * 512:HALO + (th + 1) * 512], start=(kc == 0), stop=(kc == KC - 1)),
                                    reads=[("wb", sB), ("ht", kc)], writes=[("bank", bgb)])
                            bys = nbank()
                            for kc in range(8):
                                P.op("tensor", lambda e, b=bys, kc=kc, d2=d2, th=th, wgi=wgi: e.matmul(
                                    banks[b][:, :], lhsT=WG[wgi][:, kc, d2 * 128:(d2 + 1) * 128],
                                    rhs=yT[:, kc, th * 512:(th + 1) * 512], start=(kc == 0), stop=(kc == 7)),
                                    reads=[("wg", wgi), "yTall"], writes=[("bank", bys)])
                            si = sgc[0] % 3
                            sgc[0] += 1
                            ti = sgc[0] % 2
                            P.op("scalar", lambda e, b=bgb, si=si, dc=dc: e.activation(
                                out=sg[si], in_=banks[b][:, :], func=AF.Sigmoid,
                                bias=cols[:, C_BG + 16 + dc:C_BG + 16 + dc + 1], scale=1.0),
                                reads=[("bank", bgb), "cols"], writes=[("sg", si)])
                            P.op("vector", lambda e, b=bys, si=si, ti=ti: e.tensor_tensor(
                                out=t2[ti], in0=banks[b][:, :], in1=sg[si], op=ALU.mult),
                                reads=[("bank", bys), ("sg", si)], writes=[("t2", ti)])
                            P.op("vector", lambda e, ti=ti, dc=dc, dc4=dc4, th=th: e.tensor_tensor(
                                out=mergedT[:, dc, th * 512:(th + 1) * 512], in0=t2[ti],
                                in1=m1[:, dc4, th * 512:(th + 1) * 512], op=ALU.add),
                                reads=[("t2", ti), ("m1", dc4, th)], writes=[("merged", dc, th)])
            dump("mergedT", mergedT, [128, KC, T], [("merged", dc, th) for dc in range(KC) for th in range(2)])
            dump("m1", m1, [128, 4, T], [])
            dump("yT5", yT, [128, 8, T], [])
            dump("pooledT5", pooledT, [128, 8, T], [])
            dump("t2a", t2[0], [128, 512], [])
            dump("t2b", t2[1], [128, 512], [])
            dump("sga", sg[0], [128, 512], [])
            dump("sgb", sg[1], [128, 512], [])
            dump("sgc", sg[2], [128, 512], [])
            P.barrier()

            stage(6)
            x2T = X2A.view(0, [128, KC, T], F32)
            for q in range(4):
                P.op("sync", lambda e, q=q: e.dma_start(out=x2T[:, 4 * q:4 * q + 4, :], in_=xT_v[:, 4 * q:4 * q + 4, HALO:TH]),
                     writes=[("x2", 4 * q + i, th) for i in range(4) for th in range(2)], dma="ld")
            for grp in range(4):
                slot = st_get(14 + grp)
                for nb in range(4):
                    dc = grp * 4 + nb
                    for th in range(2):
                        b = nbank()
                        for kc in range(KC):
                            P.op("tensor", lambda e, b=b, slot=slot, kc=kc, nb=nb, th=th: e.matmul(
                                banks[b][:, :], lhsT=WB(slot)[:, kc, nb * 128:(nb + 1) * 128],
                                rhs=mergedT[:, kc, th * 512:(th + 1) * 512], start=(kc == 0), stop=(kc == KC - 1)),
                                reads=[("wb", slot), "mergedall"], writes=[("bank", b)])
                        P.op("vector", lambda e, b=b, dc=dc, th=th: e.tensor_tensor(
                            out=x2T[:, dc, th * 512:(th + 1) * 512], in0=banks[b][:, :],
                            in1=x2T[:, dc, th * 512:(th + 1) * 512], op=ALU.add),
                            reads=[("bank", b), ("x2", dc, th)], writes=[("x2", dc, th)])
            dump("x2T", x2T, [128, KC, T], [("x2", dc, th) for dc in range(KC) for th in range(2)])
            P.barrier()

            stage(7)
            sq2 = [WGA.view(i * 4096, [128, T], F32) for i in range(2)]
            rstd2 = WGA.view(8192, [128, T], F32)
            rmsnorm(lambda kc: x2T[:, kc, :], lambda kc: [], T, C_G2,
                    lambda kc: HT[:, kc, HALO:TH], lambda kc: [("ht", kc)], sq2, rstd2, "n2")
            X2d_v = X2d.rearrange("(kc p) t -> p kc t", p=128)
            for q in range(4):
                P.op("sync", lambda e, q=q: e.dma_start(out=X2d_v[:, 4 * q:4 * q + 4, :], in_=x2T[:, 4 * q:4 * q + 4, :]),
                     writes=[("x2d", q)], dma="out")

            stage(8)
            qT = BIG.view(M_OFF, [128, KC, T], BF16)
            UT_v = UT_d.rearrange("(kc p) e -> p kc e", p=128)
            for grp in range(4):
                slot = st_get(18 + grp)
                if grp == 3:
                    P.op("gpsimd", lambda e: e.dma_start(out=WB(1), in_=UT_v[:, :, 0:512]), writes=[("wb", 1)], dma="wc")
                for nb in range(4):
                    qc = grp * 4 + nb
                    for th in range(2):
                        b = nbank()
                        for kc in range(KC):
                            P.op("tensor", lambda e, b=b, slot=slot, kc=kc, nb=nb, th=th: e.matmul(
                                banks[b][:, :], lhsT=WB(slot)[:, kc, nb * 128:(nb + 1) * 128],
                                rhs=HT[:, kc, HALO + th * 512:HALO + (th + 1) * 512], start=(kc == 0), stop=(kc == KC - 1)),
                                reads=[("wb", slot), ("ht", kc)], writes=[("bank", b)])
                        P.op("scalar", lambda e, b=b, qc=qc, th=th: e.copy(
                            out=qT[:, qc, th * 512:(th + 1) * 512], in_=banks[b][:, :]),
                            reads=[("bank", b)], writes=[("qT", qc, th)])
            P.barrier()

            stage(9)
            def xv(off, shape, dt):
                return X2A.view(off, shape, dt)
            s_sb = [xv(0, [128, 16, 128], F32)]
            s_scr = xv(8192, [128, 16, 128], F32)
            cand = xv(8192, [128, 8, 256], F32)
            oh = xv(16384, [128, 8, 16, 16], F32)
            t16 = xv(24576, [128, 16, 16], F32)
            i16 = xv(25600, [128, 16, 16], U32)
            i16f = xv(26624, [128, 16, 16], F32)
            cscr = [xv(27648 + i * 1024, [128, 256], F32) for i in range(2)]
            ts = xv(29696, [128, 8, 16], F32)
            pos = xv(30208, [128, 8, 16], U32)
            posf = xv(30720, [128, 8, 16], F32)
            af_ = xv(31232, [128, 8, 16], F32)
            bf_ = xv(31744, [128, 8, 16], F32)
            posa_u = xv(32256 - 512 - 512 + 0, [128, 8, 16], U32) if False else WGA.view(13824, [128, 8, 16], U32)
            posb_u = WGA.view(14336, [128, 8, 16], U32)
            zs = WGA.view(14848, [128, 8], F32)
            sel = [WGA.view(12288 + i * 512, [128, 128], F32) for i in range(3)]
            ijgT = WGA.view(0, [128, 3, T], F32)
            Ub = [BIG.view(i * 16384, [128, KC, 512], BF16) for i in range(2)]
            Ado = [BIG.view(32768 + i * 4096, [128, 2, T], BF16) for i in range(2)]
            NSR = 16
            SR = [(BIG.view(40960 + i * 256, [128, 128], BF16), BIG.view(40960 + 4096 + i * 256, [128, 128], BF16))
                  for i in range(NSR)]
            Gs = [X2A.view(32768, [128, 128, 128], BF16), X2A.view(0, [128, 128, 128], BF16)]
            UT_v = UT_d.rearrange("(kc p) e -> p kc e", p=128)
            Ad_w = Ad.rearrange("b e t -> e b t")
            Gd_v = Gd.rearrange("i j t -> j i t")
            NG1 = NEB // 4

            def s1_load_u(gi):
                P.op("gpsimd", lambda e, gi=gi: e.dma_start(out=Ub[(gi + 1) % 2], in_=UT_v[:, :, gi * 512:(gi + 1) * 512]),
                     writes=[("Ub", (gi + 1) % 2)], dma="wc")

            def s1_chains():
                for gi in range(NG1):
                    if gi + 1 < NG1:
                        s1_load_u(gi + 1)
                    for eb4 in range(4):
                        pair = gi * 2 + eb4 // 2
                        slot = pair % 2
                        for th in range(2):
                            b = nbank()
                            for kc in range(KC):
                                P.op("tensor", lambda e, b=b, gi=gi, kc=kc, eb4=eb4, th=th: e.matmul(
                                    banks[b][:, :], lhsT=Ub[(gi + 1) % 2][:, kc, eb4 * 128:(eb4 + 1) * 128],
                                    rhs=HT[:, kc, HALO + th * 512:HALO + (th + 1) * 512],
                                    start=(kc == 0), stop=(kc == KC - 1)),
                                    reads=[("Ub", (gi + 1) % 2)], writes=[("bank", b)])
                            P.op("scalar", lambda e, b=b, slot=slot, eb4=eb4, th=th: e.activation(
                                out=Ado[slot][:, eb4 % 2, th * 512:(th + 1) * 512], in_=banks[b][:, :],
                                func=AF.Gelu_apprx_tanh),
                                reads=[("bank", b)], writes=[("Ado", slot, eb4 % 2, th)])
                            if eb4 % 2 == 1 and th == 1:
                                P.op("sync", lambda e, slot=slot, pair=pair: e.dma_start(
                                    out=Ad_w[:, pair * 2:pair * 2 + 2, :], in_=Ado[slot]),
                                    reads=[("Ado", slot, a, c) for a in range(2) for c in range(2)],
                                    writes=[("Ad", pair)], dma="ww")
                            yield

            s1_it = s1_chains()

            def s1_step(n):
                for _ in range(n):
                    if next(s1_it, "done") == "done":
                        return

            def transposes(tt):
                bt = nbank()
                for w3 in range(3):
                    P.op("tensor", lambda e, w3=w3, bt=bt: e.transpose(out=banks[bt][:, w3 * 128:(w3 + 1) * 128], in_=sel[w3],
                                                                       identity=ident),
                         reads=[("sel", w3), "ident"], writes=[("bank", bt)])
                P.op("scalar", lambda e, bt=bt, tt=tt: e.copy(
                    out=ijgT[:, :, tt * 128:(tt + 1) * 128], in_=banks[bt][:, 0:384].rearrange("p (a t) -> p a t", a=3)),
                    reads=[("bank", bt)], writes=[("ijgT", tt)])

            prev_cand_readers = [(k_, h_) for k_ in ("tsa", "tsb", "posa", "posb") for h_ in range(8)]
            for tt in range(8):
                r = 0
                sb_ = [nbank() for _ in range(4)]
                for hh in range(16):
                    b = sb_[hh // 4]
                    P.op("tensor", lambda e, b=b, hh=hh, tt=tt: e.matmul(
                        banks[b][:, (hh % 4) * 128:(hh % 4 + 1) * 128], lhsT=qT[:, hh, tt * 128:(tt + 1) * 128],
                        rhs=keysT[:, hh, :], start=True, stop=True),
                        reads=["keysT", "qTall"], writes=[("bank", b)])
                for q4 in range(4):
                    P.op("scalar", lambda e, q4=q4, r=r, b=sb_[q4]: e.copy(
                        out=s_sb[r][:, q4 * 4:(q4 + 1) * 4, :], in_=banks[b][:, :].rearrange("p (a n) -> p a n", a=4)),
                        reads=[("bank", sb_[q4])], writes=[("s_sb", r, q4)])
                if tt > 0:
                    transposes(tt - 1)
                for hh in range(16):
                    P.op("vector", lambda e, hh=hh, r=r: e.max(out=t16[:, hh, 0:8], in_=s_sb[r][:, hh, :]),
                         reads=[("s_sb", r, hh // 4)], writes=[("t16a", hh)])
                for hh in range(16):
                    P.op("vector", lambda e, hh=hh, r=r: e.max_index(out=i16[:, hh, 0:8], in_max=t16[:, hh, 0:8],
                                                                     in_values=s_sb[r][:, hh, :]),
                         reads=[("s_sb", r, hh // 4), ("t16a", hh)], writes=[("i16a", hh)])
                for hh in range(16):
                    P.op("vector", lambda e, hh=hh, r=r: e.match_replace(out=s_scr[:, hh, :], in_to_replace=t16[:, hh, 0:8],
                                                                         in_values=s_sb[r][:, hh, :], imm_value=-1e30),
                         reads=[("s_sb", r, hh // 4), ("t16a", hh), "cand"] + prev_cand_readers, writes=[("s_scr", hh)])
                for hh in range(16):
                    P.op("vector", lambda e, hh=hh: e.max(out=t16[:, hh, 8:16], in_=s_scr[:, hh, :]),
                         reads=[("s_scr", hh)], writes=[("t16b", hh)])
                for hh in range(16):
                    P.op("vector", lambda e, hh=hh: e.max_index(out=i16[:, hh, 8:16], in_max=t16[:, hh, 8:16],
                                                                in_values=s_scr[:, hh, :]),
                         reads=[("s_scr", hh), ("t16b", hh)], writes=[("i16b", hh)])
                allt = [("t16a", hh) for hh in range(16)] + [("t16b", hh) for hh in range(16)]
                alli = [("i16a", hh) for hh in range(16)] + [("i16b", hh) for hh in range(16)]
                P.op("vector", lambda e: e.tensor_copy(out=i16f, in_=i16), reads=alli, writes=["i16f"])
                P.op("vector", lambda e: e.tensor_tensor(
                    out=cand.rearrange("p h (a b) -> p h a b", a=16),
                    in0=ap_of(t16, 0, [[32, 8], [1, 16], [0, 16]]),
                    in1=ap_of(t16, 16, [[32, 8], [0, 16], [1, 16]]), op=ALU.add),
                    reads=allt + alli + [("s_scr", hh) for hh in range(16)], writes=["cand"])
                for h in range(8):
                    P.op("vector", lambda e, h=h: e.max(out=ts[:, h, 0:8], in_=cand[:, h, :]),
                         reads=["cand"], writes=[("tsa", h)])
                for h in range(8):
                    P.op("vector", lambda e, h=h: e.max_index(out=pos[:, h, 0:8], in_max=ts[:, h, 0:8], in_values=cand[:, h, :]),
                         reads=["cand", ("tsa", h)], writes=[("posa", h)])
                for h in range(8):
                    c_ = cscr[h % 2]
                    P.op("vector", lambda e, h=h, c_=c_: e.match_replace(out=c_, in_to_replace=ts[:, h, 0:8],
                                                                         in_values=cand[:, h, :], imm_value=-1e30),
                         reads=["cand", ("tsa", h)], writes=[("cscr", h % 2)])
                    P.op("vector", lambda e, h=h, c_=c_: e.max(out=ts[:, h, 8:16], in_=c_),
                         reads=[("cscr", h % 2)], writes=[("tsb", h)])
                    P.op("vector", lambda e, h=h, c_=c_: e.max_index(out=pos[:, h, 8:16], in_max=ts[:, h, 8:16], in_values=c_),
                         reads=[("cscr", h % 2), ("tsb", h)], writes=[("posb", h)])
                allts = [("tsa", h) for h in range(8)] + [("tsb", h) for h in range(8)]
                allpos = [("posa", h) for h in range(8)] + [("posb", h) for h in range(8)]
                P.op("vector", lambda e: e.tensor_single_scalar(out=posa_u, in_=pos, scalar=4, op=ALU.logical_shift_right),
                     reads=allpos, writes=["pa_u"])
                P.op("vector", lambda e: e.tensor_single_scalar(out=posb_u, in_=pos, scalar=15, op=ALU.bitwise_and),
                     reads=allpos, writes=["pb_u"])
                P.op("vector", lambda e: e.tensor_copy(out=af_, in_=posa_u), reads=["pa_u"], writes=["af"])
                P.op("vector", lambda e: e.tensor_copy(out=bf_, in_=posb_u), reads=["pb_u"], writes=["bf"])
                for which, src, off, dst in ((0, af_, 0, sel[0]), (1, bf_, 16, sel[1])):
                    P.op("vector", lambda e, src=src: e.tensor_tensor(
                        out=oh, in0=ap_of(src, 0, [[16, 8], [1, 16], [0, 16]]),
                        in1=ap_of(iota_f, 0, [[0, 8], [0, 16], [1, 16]]), op=ALU.is_equal),
                        reads=["af", "bf", "iota"], writes=["oh0", "oh1"])
                    P.op("vector", lambda e, off=off: e.tensor_tensor(
                        out=oh, in0=oh, in1=ap_of(i16f, off, [[32, 8], [0, 16], [1, 16]]), op=ALU.mult),
                        reads=["oh0", "i16f"], writes=["oh1"])
                    P.op("vector", lambda e, dst=dst: e.tensor_reduce(
                        out=dst.rearrange("p (h k) -> p h k", h=8), in_=oh, axis=AX.X, op=ALU.add),
                        reads=["oh1"], writes=[("sel", which)])
                P.op("vector", lambda e: e.tensor_tensor(
                    out=posf, in0=ts, in1=ap_of(ts, 0, [[16, 8], [0, 16]]), op=ALU.subtract),
                    reads=allts, writes=["tsd", "tse"])
                s1_step(S1_PER_TT - 4)
                P.op("scalar", lambda e: e.activation(out=posf, in_=posf, func=AF.Exp), reads=["tsd"], writes=["tse"])
                P.op("vector", lambda e: e.tensor_reduce(out=zs, in_=posf, axis=AX.X, op=ALU.add),
                     reads=["tse"], writes=["zs0", "zs"])
                P.op("vector", lambda e: e.reciprocal(out=zs, in_=zs), reads=["zs0"], writes=["zs"])
                P.op("vector", lambda e: e.tensor_tensor(
                    out=sel[2].rearrange("p (h k) -> p h k", h=8), in0=posf, in1=ap_of(zs, 0, [[1, 8], [0, 16]]), op=ALU.mult),
                    reads=["tse", "zs"], writes=[("sel", 2)])
                s1_step(4)
            transposes(7)
            Vr0 = BIG.view(M_OFF, [128, 8, D], BF16)
            V_v0 = V_d.rearrange("(b e) d -> e b d", e=128)
            for hf in range(2):
                P.op("gpsimd", lambda e, hf=hf: e.dma_start(
                    out=Vr0[:, hf * 4:(hf + 1) * 4, :], in_=V_v0[:, hf * 4:(hf + 1) * 4, :]),
                    writes=["qTall", ("Vr", 0, hf)], dma="wc")
            dump("ijgT", ijgT, [128, 3, T], [("ijgT", tt) for tt in range(8)])
            tokc = [0]
            for tt in range(8):
                g = tt % 2
                for q in range(32):
                    b = nbank()
                    for t4 in range(4):
                        tok = q * 4 + t4
                        t = tt * 128 + tok
                        si = tokc[0] % NSR
                        tokc[0] += 1
                        S_, R_ = SR[si]
                        P.op("vector", lambda e, S_=S_, t=t: e.tensor_scalar(
                            out=S_, in0=iota_b, scalar1=ijgT[:, 1, t:t + 1], scalar2=None, op0=ALU.is_equal),
                            reads=["iota_b", ("ijgT", tt)], writes=[("S", si)])
                        P.op("vector", lambda e, R_=R_, t=t: e.tensor_scalar(
                            out=R_, in0=iota_b, scalar1=ijgT[:, 0, t:t + 1], scalar2=ijgT[:, 2, t:t + 1],
                            op0=ALU.is_equal, op1=ALU.mult),
                            reads=["iota_b", ("ijgT", tt)], writes=[("R", si)])
                        P.op("tensor", lambda e, b=b, t4=t4, S_=S_, R_=R_: e.matmul(
                            banks[b][:, t4 * 128:(t4 + 1) * 128], lhsT=S_, rhs=R_, start=True, stop=True),
                            reads=[("S", si), ("R", si)], writes=[("bank", b)])
                    P.op("scalar", lambda e, b=b, g=g, q=q: e.copy(
                        out=Gs[g][:, :, q * 4:(q + 1) * 4],
                        in_=ap_of(banks[b][:, :], 0, [[1, 128], [128, 4]])),
                        reads=[("bank", b)], writes=[("Gs", g, q)])
                    if q % 2 == 1:
                        s1_step(1)
                for i8 in range(8):
                    P.op("sync", lambda e, g=g, i8=i8, tt=tt: e.dma_start(
                        out=Gd_v[:, i8 * 16:(i8 + 1) * 16, tt * 128:(tt + 1) * 128], in_=Gs[g][:, i8 * 16:(i8 + 1) * 16, :]),
                        reads=[("Gs", g, q) for q in range(32)], writes=[("Gd", tt, i8)], dma="gw")
            s1_step(10 ** 6)
            P.barrier()

            stage(12)
            Vr = [BIG.view(M_OFF, [128, 8, D], BF16), BIG.view(0, [128, 8, D], BF16)]
            Ar = [BIG.view(32768, [128, 8, T], BF16), HTA.view(0, [128, 8, T], BF16)]
            Gr = [HTA.view(16384, [128, 8, T], BF16), WGA.view(0, [128, 8, T], BF16)]
            V_v = V_d.rearrange("(b e) d -> e b d", e=128)
            Ad_r = Ad.rearrange("b e t -> e b t")
            Gd_r = Gd.rearrange("i j t -> j i t")
            NG2 = NEB // 8

            def s2_load(gi, extra_reads=()):
                s_ = gi % 2
                for hf in range(2 if gi > 0 else 0):
                    P.op("gpsimd", lambda e, s_=s_, gi=gi, hf=hf: e.dma_start(
                        out=Vr[s_][:, hf * 4:(hf + 1) * 4, :], in_=V_v[:, gi * 8 + hf * 4:gi * 8 + (hf + 1) * 4, :]),
                        reads=list(extra_reads), writes=[("Vr", s_, hf)], dma="wc")
                P.op("sync", lambda e, s_=s_, gi=gi: e.dma_start(out=Ar[s_], in_=Ad_r[:, gi * 8:(gi + 1) * 8, :]),
                     reads=list(extra_reads), writes=[("Ar", s_, 0), ("Ar", s_, 1), ("W", s_, 0), ("W", s_, 1)], dma="wr")
                P.op("sync", lambda e, s_=s_, gi=gi: e.dma_start(out=Gr[s_], in_=Gd_r[:, gi * 8:(gi + 1) * 8, :]),
                     reads=list(extra_reads), writes=[("Gr", s_)], dma="gr")

            def s2_mult(gi):
                s_ = gi % 2
                for hf in range(2):
                    P.op("vector", lambda e, s_=s_, hf=hf: e.tensor_tensor(
                        out=Ar[s_][:, hf * 4:(hf + 1) * 4, :], in0=Ar[s_][:, hf * 4:(hf + 1) * 4, :],
                        in1=Gr[s_][:, hf * 4:(hf + 1) * 4, :], op=ALU.mult),
                        reads=[("Ar", s_, hf), ("Gr", s_)], writes=[("Ar", s_, hf), ("W", s_, hf)])

            s2_load(0)
            P.op("sync", lambda e: e.dma_start(out=x2T[:, 0:4, :], in_=X2d_v[:, 0:4, :]), writes=[("x2r", 0)], dma="ld")
            s2_mult(0)
            for q in range(1, 4):
                P.op("sync", lambda e, q=q: e.dma_start(out=x2T[:, 4 * q:4 * q + 4, :], in_=X2d_v[:, 4 * q:4 * q + 4, :]),
                     reads=[("W", 0, 0)], writes=[("x2r", q)], dma="ld")
            s2_load(1, extra_reads=[("W", 0, 0)])
            rnd = [0]
            for gi in range(NG2):
                s_ = gi % 2
                for r8 in range(8):
                    if r8 == 6 and gi + 1 < NG2:
                        s2_mult(gi + 1)
                    base = (rnd[0] % 2) * 4
                    rnd[0] += 1
                    for k in range(4):
                        dc = r8 * 2 + k // 2
                        th = k % 2
                        b = base + k
                        for e8 in range(8):
                            P.op("tensor", lambda e, b=b, s_=s_, e8=e8, dc=dc, th=th: e.matmul(
                                banks[b][:, :], lhsT=Vr[s_][:, e8, dc * 128:(dc + 1) * 128],
                                rhs=Ar[s_][:, e8, th * 512:(th + 1) * 512], start=(e8 == 0), stop=(e8 == 7)),
                                reads=[("Vr", s_, e8 // 4), ("W", s_, e8 // 4)], writes=[("bank", b)])
                        P.op("vector", lambda e, b=b, dc=dc, th=th: e.tensor_tensor(
                            out=x2T[:, dc, th * 512:(th + 1) * 512], in0=banks[b][:, :],
                            in1=x2T[:, dc, th * 512:(th + 1) * 512], op=ALU.add),
                            reads=[("bank", b), ("x2r", dc // 4)], writes=[("x3", dc, th)])
                if gi + 2 < NG2:
                    s2_load(gi + 2)
            P.barrier()

            stage(13)
            sq3 = [WGA.view(i * 4096, [128, T], F32) for i in range(2)]
            rmsnorm(lambda kc: x2T[:, kc, :], lambda kc: [], T, C_GF,
                    lambda kc: x2T[:, kc, :], lambda kc: [("o", kc)], sq3, rstd2, "n3")
            out_v = out_d.rearrange("(kc p) t -> p kc t", p=128)
            for q in range(4):
                P.op("sync", lambda e, q=q: e.dma_start(out=out_v[:, 4 * q:4 * q + 4, :], in_=x2T[:, 4 * q:4 * q + 4, :]),
                     reads=[("o", 4 * q + i) for i in range(4)], writes=[("out", q)], dma="out")

        except _Stop:
            pass
        P.barrier()
        P.op("sync", None)
        P.emit(sems, block)
    return nc, list(dbg_out.keys())


def prepare_inputs(inputs, ncores=NCORES):
    f = np.float32
    x = np.asarray(inputs["x"], f)[0]
    xpad = np.concatenate([np.zeros((HALO, D), f), x], axis=0)
    w_s = np.asarray(inputs["w_s"], f)
    wsT = np.ascontiguousarray(w_s.transpose(2, 0, 1).reshape(128, 1024))
    s_idx = np.arange(128)[:, None]
    t_idx = np.arange(128)[None, :]
    maskT = np.tile((s_idx <= t_idx).astype(f), (1, 8))
    cols = np.zeros((128, NCOLS), f)
    cols[:, C_G1:C_G1 + 16] = np.asarray(inputs["norm1_g"], f).reshape(16, 128).T
    cols[:, C_G2:C_G2 + 16] = np.asarray(inputs["norm2_g"], f).reshape(16, 128).T
    cols[:, C_GF:C_GF + 16] = np.asarray(inputs["final_g"], f).reshape(16, 128).T
    cols[:, C_BG:C_BG + 32] = np.asarray(inputs["b_gate"], f).reshape(32, 128).T
    cols[:, C_PS:C_PS + 8] = np.asarray(inputs["pool_scale"], f).reshape(8, 128).T
    cols[:, C_EPS] = EPS
    keysT = np.ascontiguousarray(np.asarray(inputs["sub_keys"], f).reshape(16, 128, 128).transpose(2, 0, 1).reshape(128, 2048))
    UT = np.ascontiguousarray(np.asarray(inputs["expert_u"], f).T)
    shared = {
        "w_in": np.ascontiguousarray(inputs["w_in"], f),
        "w_out": np.ascontiguousarray(inputs["w_out"], f),
        "w_q": np.ascontiguousarray(inputs["w_q"], f),
        "w_gproj": np.ascontiguousarray(inputs["w_gproj"], f),
        "w_pool": np.ascontiguousarray(inputs["w_pool"], f),
        "wsT": wsT, "maskT": maskT,
        "bs_row": np.ascontiguousarray(np.asarray(inputs["b_s"], f).reshape(1, 1024)),
        "cols": cols,
        "lng": np.ascontiguousarray(np.asarray(inputs["ln_v_g"], f).reshape(1, 1024)),
        "lnb": np.ascontiguousarray(np.asarray(inputs["ln_v_b"], f).reshape(1, 1024)),
        "keysT": keysT, "UT": UT, "V": np.ascontiguousarray(inputs["expert_v"], f),
        "ident": np.eye(128, dtype=f), "ones": np.ones((128, 128), f),
        "iota": np.tile(np.arange(128, dtype=f)[None, :], (128, 1)),
    }
    in_maps = []
    for c in range(ncores):
        m = dict(shared)
        m["xT"] = np.ascontiguousarray(xpad[c * T:c * T + TH].T)
        pos1 = np.arange(c * T + 1, c * T + 17, dtype=f)
        m["invc"] = np.concatenate([1.0 / np.minimum(pos1, float(w)) for w in (2, 4, 8, 16)]).reshape(1, 64).astype(f)
        in_maps.append(m)
    return in_maps


_CACHE = {}


def kernel(**inputs):
    if "nc" not in _CACHE:
        _CACHE["nc"] = build_program()[0]
    nc = _CACHE["nc"]
    in_maps = prepare_inputs(inputs)
    res = run_bass_kernel_spmd(nc, in_maps, core_ids=list(range(NCORES)))
    outT = [np.asarray(r["outT"]) for r in res.results]
    out = np.concatenate([o.T for o in outT], axis=0)
    return np.ascontiguousarray(out.reshape(1, NCORES * T, D).astype(np.float32))
```
